# Optimizing a Trainium2 kernel written in Bass

```python
import jax, jax.numpy as jnp
from jax import lax
import numpy as np


D_MODEL = 2048
BATCH = 4
SEQ = 4096
DEPTH = 1

M_HEADS = 4
M_WIDTH = D_MODEL // 2
M_HEAD_DIM = M_WIDTH // M_HEADS
M_CHUNK = 128
CONV_WIDTH = 4
G_WIDTH = D_MODEL // 2
G_GROUPS = 8
G_GROUP_DIM = G_WIDTH // G_GROUPS
G_CHUNK = 128
N_GROUPS = 4
EXPERTS_PER_GROUP = 8
N_EXPERTS = N_GROUPS * EXPERTS_PER_GROUP
TOP_K = 2
D_EXPERT = 512
MOE_BLOCK = 128
PLE_DIM = 256
N_BRANCHES = 2
EPS = 1e-6
IN_COLS = 4 * M_WIDTH + 2 * M_HEADS + 2 * G_WIDTH + N_BRANCHES * D_MODEL

kernel_name = 'hybrid_mlstm_gmlp_hmoe_block'


def rms_norm(x, g):
    xf = x.astype(jnp.float32)
    y = xf * lax.rsqrt(jnp.mean(xf * xf, axis=-1, keepdims=True) + EPS)
    return (y * g.astype(jnp.float32)).astype(x.dtype)


def causal_conv(x, w, b):
    K, C = w.shape
    y = lax.conv_general_dilated(x, w[:, None, :].astype(x.dtype), window_strides=(1,),
                                 padding=[(K - 1, 0)], dimension_numbers=('NWC', 'WIO', 'NWC'),
                                 feature_group_count=C)
    return y + b


def mlstm_chunkwise(q, k, v, i_pre, f_pre):
    B, S, H, Dh = q.shape
    L = M_CHUNK
    NC = S // L
    f32 = jnp.float32

    def chunk(t):
        t = t.astype(f32).reshape((B, NC, L, H) + t.shape[3:])
        return jnp.moveaxis(t, 3, 1)

    q = chunk(q)
    k = chunk(k) * (Dh ** -0.5)
    v = chunk(v)
    ig = chunk(i_pre)
    lf = jax.nn.log_sigmoid(chunk(f_pre))
    b = jnp.cumsum(lf, axis=-1)
    g = b[..., -1]

    a = g[..., None] - b + ig
    m_loc = jnp.max(a, axis=-1)
    w_loc = jnp.exp(a - m_loc[..., None])
    kv_loc = jnp.einsum('bhcl,bhcld,bhcle->bhcde', w_loc, k, v)
    n_loc = jnp.einsum('bhcl,bhcld->bhcd', w_loc, k)

    def step(carry, xs):
        C, n, m = carry
        kv_c, n_c, m_c, g_c = xs
        m_new = jnp.maximum(g_c + m, m_c)
        s_old = jnp.exp(g_c + m - m_new)
        s_loc = jnp.exp(m_c - m_new)
        C_new = s_old[..., None, None] * C + s_loc[..., None, None] * kv_c
        n_new = s_old[..., None] * n + s_loc[..., None] * n_c
        return (C_new, n_new, m_new), (C, n, m)

    init = (jnp.zeros((B, H, Dh, Dh), f32), jnp.zeros((B, H, Dh), f32), jnp.zeros((B, H), f32))
    xs = (jnp.moveaxis(kv_loc, 2, 0), jnp.moveaxis(n_loc, 2, 0),
          jnp.moveaxis(m_loc, 2, 0), jnp.moveaxis(g, 2, 0))
    _, (C_prev, n_prev, m_prev) = lax.scan(step, init, xs)
    C_prev = jnp.moveaxis(C_prev, 0, 2)
    n_prev = jnp.moveaxis(n_prev, 0, 2)
    m_prev = jnp.moveaxis(m_prev, 0, 2)

    log_d = b[..., :, None] - b[..., None, :] + ig[..., None, :]
    causal = jnp.tril(jnp.ones((L, L), dtype=bool))
    log_d = jnp.where(causal, log_d, -jnp.inf)
    log_inter = b + m_prev[..., None]
    m = jnp.maximum(log_inter, jnp.max(log_d, axis=-1))
    d = jnp.exp(log_d - m[..., None])
    s_inter = jnp.exp(log_inter - m)
    qk = jnp.einsum('bhcld,bhcsd->bhcls', q, k) * d
    num = (jnp.einsum('bhcls,bhcse->bhcle', qk, v)
           + s_inter[..., None] * jnp.einsum('bhcld,bhcde->bhcle', q, C_prev))
    den = jnp.sum(qk, axis=-1) + s_inter * jnp.einsum('bhcld,bhcd->bhcl', q, n_prev)
    h = num / jnp.maximum(jnp.abs(den), jnp.exp(-m))[..., None]
    return jnp.moveaxis(h, 1, 3).reshape(B, S, H, Dh)


def head_norm(h, g):
    B, S, H, Dh = h.shape
    mu = jnp.mean(h, axis=-1, keepdims=True)
    var = jnp.mean(jnp.square(h - mu), axis=-1, keepdims=True)
    return ((h - mu) * lax.rsqrt(var + EPS)).reshape(B, S, H * Dh) * g.astype(jnp.float32)


def spatial_gating(u, v, ln_g, ln_b, w_s, b_s):
    B, S, _ = v.shape
    NCk = S // G_CHUNK
    vf = v.astype(jnp.float32)
    mu = jnp.mean(vf, axis=-1, keepdims=True)
    var = jnp.mean(jnp.square(vf - mu), axis=-1, keepdims=True)
    vn = (vf - mu) * lax.rsqrt(var + EPS) * ln_g.astype(jnp.float32) + ln_b.astype(jnp.float32)
    vn = vn.reshape(B, NCk, G_CHUNK, G_GROUPS, G_GROUP_DIM)
    causal = jnp.tril(jnp.ones((G_CHUNK, G_CHUNK), dtype=bool))
    w = jnp.where(causal, w_s.astype(jnp.float32), 0.0)
    mixed = (jnp.einsum('gts,bcsge->bctge', w, vn)
             + jnp.transpose(b_s.astype(jnp.float32))[None, None, :, :, None])
    return u * mixed.reshape(B, S, G_WIDTH).astype(u.dtype)


def token_mixers(hn, w_in, conv_w, conv_b, b_gate, gn_m, ln_g, ln_b, w_s, b_s, w_bm, w_bg, w_out):
    B, S, _ = hn.shape
    proj = hn @ w_in
    sizes = (M_WIDTH, M_WIDTH, M_WIDTH, M_WIDTH, 2 * M_HEADS, G_WIDTH, G_WIDTH)
    cuts = [int(c) for c in np.cumsum(sizes)]
    q, k, v, o_pre, if_pre, u, vg, gates = jnp.split(proj, cuts, axis=-1)
    qk = jax.nn.silu(causal_conv(jnp.concatenate([q, k], axis=-1), conv_w, conv_b))
    q, k = jnp.split(qk, 2, axis=-1)
    heads = lambda t: t.reshape(B, S, M_HEADS, M_HEAD_DIM)
    gate_pre = if_pre.astype(jnp.float32) + b_gate.astype(jnp.float32)
    h = mlstm_chunkwise(heads(q), heads(k), heads(v), gate_pre[..., :M_HEADS], gate_pre[..., M_HEADS:])
    h_m = (jax.nn.sigmoid(o_pre.astype(jnp.float32)) * head_norm(h, gn_m)).astype(hn.dtype)
    h_g = spatial_gating(jax.nn.gelu(u), jax.nn.gelu(vg), ln_g, ln_b, w_s, b_s)
    g_m, g_g = jnp.split(jax.nn.sigmoid(gates), 2, axis=-1)
    merged = g_m * (h_m @ w_bm) + g_g * (h_g @ w_bg)
    return merged @ w_out


def hier_moe(h, w_rg, b_rg, w_re, b_re, w1, w3, w2):
    B, S, D = h.shape
    T = B * S
    f32 = jnp.float32
    hf = h.reshape(T, D)
    g_logits = (hf @ w_rg).astype(f32) + b_rg.astype(f32)
    g_prob = jax.nn.softmax(g_logits, axis=-1)
    grp = jnp.argmax(g_logits, axis=-1)
    p_grp = jnp.take_along_axis(g_prob, grp[:, None], axis=-1)
    e_logits = ((hf @ w_re).astype(f32) + b_re.astype(f32)).reshape(T, N_GROUPS, EXPERTS_PER_GROUP)
    e_logits = jnp.take_along_axis(e_logits, grp[:, None, None], axis=1)[:, 0]
    top_v, top_i = lax.top_k(e_logits, TOP_K)
    wts = jax.nn.softmax(top_v, axis=-1) * p_grp
    eid = grp[:, None] * EXPERTS_PER_GROUP + top_i

    A = T * TOP_K
    flat_e = eid.reshape(A)
    flat_w = wts.reshape(A)
    flat_t = jnp.repeat(jnp.arange(T, dtype=jnp.int32), TOP_K)
    order = jnp.argsort(flat_e)
    se, st, sw = flat_e[order], flat_t[order], flat_w[order]
    counts = jnp.bincount(flat_e, length=N_EXPERTS)
    starts = jnp.cumsum(counts) - counts
    padded = (counts + MOE_BLOCK - 1) // MOE_BLOCK * MOE_BLOCK
    pends = jnp.cumsum(padded)
    pstarts = pends - padded
    dest = pstarts[se] + jnp.arange(A) - starts[se]
    NB = -(-A // MOE_BLOCK) + N_EXPERTS
    P = NB * MOE_BLOCK
    tok_buf = jnp.full((P,), T, dtype=jnp.int32).at[dest].set(st)
    w_buf = jnp.zeros((P,), f32).at[dest].set(sw)
    blk_e = jnp.clip(jnp.searchsorted(pends, jnp.arange(NB) * MOE_BLOCK, side='right'), 0, N_EXPERTS - 1)

    h_pad = jnp.concatenate([hf, jnp.zeros((1, D), hf.dtype)], axis=0)
    xb = h_pad[tok_buf].reshape(NB, MOE_BLOCK, D)

    def expert_block(args):
        xblk, e = args
        return (jax.nn.silu(xblk @ w1[e]) * (xblk @ w3[e])) @ w2[e]

    yb = lax.map(expert_block, (xb, blk_e)).reshape(P, D)
    out = jnp.zeros((T + 1, D), f32).at[tok_buf].add(yb.astype(f32) * w_buf[:, None])[:T]
    return out.reshape(B, S, D).astype(h.dtype)


def setup_inputs(seed: int = 0) -> dict:
    key = jax.random.key(seed)
    ks = jax.random.split(key, 32)
    f32 = jnp.float32
    nrm = lambda k, shape, scale: jax.random.normal(k, shape, f32) * scale
    L = DEPTH
    x = nrm(ks[0], (BATCH, SEQ, D_MODEL), 1.0)
    p = nrm(ks[1], (DEPTH, BATCH, SEQ, PLE_DIM), 1.0)
    g_mix = 1.0 + nrm(ks[2], (L, D_MODEL), 0.02)
    w_in = nrm(ks[3], (L, D_MODEL, IN_COLS), D_MODEL ** -0.5)
    conv_w = nrm(ks[4], (L, CONV_WIDTH, 2 * M_WIDTH), CONV_WIDTH ** -0.5)
    conv_b = nrm(ks[5], (L, 2 * M_WIDTH), 0.02)
    b_i = nrm(ks[6], (L, M_HEADS), 0.1)
    b_f = jnp.linspace(3.0, 6.0, M_HEADS, dtype=f32)[None, :] + nrm(ks[7], (L, M_HEADS), 0.1)
    b_gate = jnp.concatenate([b_i, b_f], axis=-1)
    gn_m = 1.0 + nrm(ks[8], (L, M_WIDTH), 0.02)
    ln_g = 1.0 + nrm(ks[9], (L, G_WIDTH), 0.02)
    ln_b = nrm(ks[10], (L, G_WIDTH), 0.02)
    w_s = nrm(ks[11], (L, G_GROUPS, G_CHUNK, G_CHUNK), G_CHUNK ** -0.5)
    b_s = 1.0 + nrm(ks[12], (L, G_GROUPS, G_CHUNK), 0.02)
    w_bm = nrm(ks[13], (L, M_WIDTH, D_MODEL), M_WIDTH ** -0.5)
    w_bg = nrm(ks[14], (L, G_WIDTH, D_MODEL), G_WIDTH ** -0.5)
    w_out = nrm(ks[15], (L, D_MODEL, D_MODEL), D_MODEL ** -0.5)
    g_ffn = 1.0 + nrm(ks[16], (L, D_MODEL), 0.02)
    w_rg = nrm(ks[17], (L, D_MODEL, N_GROUPS), D_MODEL ** -0.5)
    b_rg = nrm(ks[18], (L, N_GROUPS), 0.01)
    w_re = nrm(ks[19], (L, D_MODEL, N_EXPERTS), D_MODEL ** -0.5)
    b_re = nrm(ks[20], (L, N_EXPERTS), 0.01)
    w1 = nrm(ks[21], (L, N_EXPERTS, D_MODEL, D_EXPERT), D_MODEL ** -0.5)
    w3 = nrm(ks[22], (L, N_EXPERTS, D_MODEL, D_EXPERT), D_MODEL ** -0.5)
    w2 = nrm(ks[23], (L, N_EXPERTS, D_EXPERT, D_MODEL), D_EXPERT ** -0.5)
    g_ple = 1.0 + nrm(ks[24], (L, D_MODEL), 0.02)
    w_ple_up = nrm(ks[25], (L, PLE_DIM, D_MODEL), PLE_DIM ** -0.5)
    w_ple_gate = nrm(ks[26], (L, D_MODEL, D_MODEL), D_MODEL ** -0.5)
    g_final = 1.0 + nrm(ks[27], (D_MODEL,), 0.02)
    return {'x': x, 'p': p, 'g_mix': g_mix, 'w_in': w_in, 'conv_w': conv_w, 'conv_b': conv_b,
            'b_gate': b_gate, 'gn_m': gn_m, 'ln_g': ln_g, 'ln_b': ln_b, 'w_s': w_s, 'b_s': b_s,
            'w_bm': w_bm, 'w_bg': w_bg, 'w_out': w_out, 'g_ffn': g_ffn, 'w_rg': w_rg, 'b_rg': b_rg,
            'w_re': w_re, 'b_re': b_re, 'w1': w1, 'w3': w3, 'w2': w2, 'g_ple': g_ple,
            'w_ple_up': w_ple_up, 'w_ple_gate': w_ple_gate, 'g_final': g_final}


def reference(x, p, g_mix, w_in, conv_w, conv_b, b_gate, gn_m, ln_g, ln_b, w_s, b_s,
              w_bm, w_bg, w_out, g_ffn, w_rg, b_rg, w_re, b_re, w1, w3, w2,
              g_ple, w_ple_up, w_ple_gate, g_final):
    for i in range(DEPTH):
        x = x + token_mixers(rms_norm(x, g_mix[i]), w_in[i], conv_w[i], conv_b[i], b_gate[i], gn_m[i],
                             ln_g[i], ln_b[i], w_s[i], b_s[i], w_bm[i], w_bg[i], w_out[i])
        x = x + hier_moe(rms_norm(x, g_ffn[i]), w_rg[i], b_rg[i], w_re[i], b_re[i], w1[i], w3[i], w2[i])
        ple = p[i] @ w_ple_up[i]
        x = x + jax.nn.sigmoid(rms_norm(x, g_ple[i]) @ w_ple_gate[i]) * ple
    return rms_norm(x, g_final)
```

```python
import contextlib
import numpy as np
import ml_dtypes
import concourse.bass as bass
import concourse.mybir as mybir
from concourse.bass_utils import run_bass_kernel_spmd

F32 = mybir.dt.float32
BF16 = mybir.dt.bfloat16
I32 = mybir.dt.int32
AF = mybir.ActivationFunctionType
ALU = mybir.AluOpType

D = 2048
NT = 2048
NCH = 16
KD = 16
EPS = 1e-6
CAP = 256
NSLOT = 32 * CAP
IN_COLS = 10248
C_Q, C_K, C_V, C_O, C_IF, C_U, C_VG, C_GM, C_GG = 0, 1024, 2048, 3072, 4096, 4104, 5128, 6152, 8200
HT = 3 + NT
VW = 264


class Res:
    __slots__ = ("name", "w", "r", "pr")

    def __init__(self, name=""):
        self.name = name
        self.w = {}
        self.r = {}
        self.pr = {}


class KB:
    def __init__(self, nc, es, ndma=(8, 8, 4)):
        self.nc = nc
        self.es = es
        self.eng = {"pe": nc.tensor, "act": nc.scalar, "dve": nc.vector,
                    "pool": nc.gpsimd, "sp": nc.sync}
        self.sem = {}
        self.cnt = {}
        self.last = {}
        self.seen = {e: {} for e in self.eng}
        for e in self.eng:
            self.sem[e] = es.enter_context(nc.semaphore("sem_" + e))
            self.cnt[e] = 0
        self.eng["cv"] = nc.gpsimd
        self.seen["cv"] = {}
        self.dq = {}
        ndma = tuple(ndma) + (8,)
        for q, n in zip(("sp", "pool", "act", "cv"), ndma):
            sems = [es.enter_context(nc.semaphore(f"dsem_{q}{i}")) for i in range(n)]
            self.dq[q] = {"sems": sems, "i": 0, "cnt": [0] * n}
            for i, s in enumerate(sems):
                self.sem[(q, i)] = s
        self.ninst = 0

    def _wait(self, e, ev):
        if ev is None:
            return
        key, val = ev
        if self.seen[e].get(key, 0) >= val:
            return
        self.eng[e].wait_ge(self.sem[key], val)
        self.seen[e][key] = val
        self.ninst += 1

    def start_fill(self, res):
        pr = dict(res.r)
        for kk, v in res.w.items():
            if pr.get(kk, 0) < v:
                pr[kk] = v
        res.pr = pr
        res.r = {}
        res.w = {}

    def _deps(self, e, reads, writes, add=False):
        for r in reads:
            for kv in list(r.w.items()):
                self._wait(e, kv)
        for w in writes:
            if not add:
                self.start_fill(w)
            for kv in list(w.pr.items()):
                self._wait(e, kv)

    def _commit(self, ev, reads, writes):
        for r in reads:
            if r.r.get(ev[0], 0) < ev[1]:
                r.r[ev[0]] = ev[1]
        for w in writes:
            if w.w.get(ev[0], 0) < ev[1]:
                w.w[ev[0]] = ev[1]

    def op(self, e, fn, reads=(), writes=(), add=False):
        self._deps(e, reads, writes, add)
        ins = fn(self.eng[e])
        self.cnt[e] += 1
        ins.then_inc(self.sem[e], 1)
        ev = (e, self.cnt[e])
        self._commit(ev, reads, writes)
        self.ninst += 1
        return ev

    def group(self, e, fns, reads=(), writes=(), add=False):
        self._deps(e, reads, writes, add)
        ins = None
        for fn in fns:
            ins = fn(self.eng[e])
            self.ninst += 1
        self.cnt[e] += 1
        ins.then_inc(self.sem[e], 1)
        ev = (e, self.cnt[e])
        self._commit(ev, reads, writes)
        return ev

    def dma(self, q, out, in_, reads=(), writes=(), fn=None, add=False):
        self._deps(q, reads, writes, add)
        d = self.dq[q]
        i = d["i"]
        d["i"] = (i + 1) % len(d["sems"])
        key = (q, i)
        if d["cnt"][i] > 0:
            self._wait(q, (key, d["cnt"][i]))
        if fn is None:
            ins = self.eng[q].dma_start(out=out, in_=in_)
        else:
            ins = fn(self.eng[q])
        d["cnt"][i] += 16
        ins.then_inc(d["sems"][i], 16)
        ev = (key, d["cnt"][i])
        self._commit(ev, reads, writes)
        self.ninst += 1
        return ev

    def barrier(self):
        evs = [(e, self.cnt[e]) for e in self.cnt if self.cnt[e] > 0]
        for q, d in self.dq.items():
            for i, c in enumerate(d["cnt"]):
                if c > 0:
                    evs.append(((q, i), c))
        for e in self.eng:
            for ev in evs:
                self._wait(e, ev)

    def sb(self, es, name, shape, dt):
        self.uid = getattr(self, "uid", 0) + 1
        return es.enter_context(self.nc.sbuf_tensor(f"sb{self.uid}_{name}", list(shape), dt))

    def ps(self, es, name, shape, dt):
        self.uid = getattr(self, "uid", 0) + 1
        return es.enter_context(self.nc.psum_tensor(f"pp{self.uid}_{name}", list(shape), dt))


def MM(out, lhsT, rhs, start, stop):
    return lambda e: e.matmul(out, lhsT=lhsT, rhs=rhs, start=start, stop=stop)


def TR(out, in_, ident):
    return lambda e: e.transpose(out=out, in_=in_, identity=ident)


def build(dbg=False, stop_after=None, CUT=None):
    nc = bass.Bass("TRN2", target_bir_lowering=False)

    def din(name, shape, dt=F32):
        return nc.dram_tensor(name, list(shape), dt, kind="ExternalInput").ap()

    def dscr(name, shape, dt):
        kind = "ExternalOutput" if dbg else "Internal"
        return nc.dram_tensor(name, list(shape), dt, kind=kind).ap()

    xm = din("xm", [NT, D]); xp = din("xp", [NT, D]); xh = din("xh", [128, D])
    flag_d = din("flag", [128, 1]); pm = din("pm", [NT, 256])
    w_in = din("w_in", [D, IN_COLS])
    convw_d = din("convw", [128, 16, 4]); convb_d = din("convb", [128, 16])
    bgate_d = din("b_gate", [8]); gnm_d = din("gn_m", [1024])
    lng_d = din("ln_g", [1024]); lnb_d = din("ln_b", [1024])
    ws_d = din("w_s", [8, 128, 128]); bs_d = din("b_s", [1024])
    wbm_d = din("w_bm", [1024, D]); wbg_d = din("w_bg", [1024, D]); wout_d = din("w_out", [D, D])
    gmix_d = din("g_mix", [D]); gffn_d = din("g_ffn", [D]); gple_d = din("g_ple", [D]); gfin_d = din("g_final", [D])
    wr_d = din("w_r", [D, 36]); br_d = din("b_r", [36])
    w1_d = din("w1", [32, D, 512]); w3_d = din("w3", [32, D, 512]); w2_d = din("w2", [32, 512, D])
    wpu_d = din("w_ple_up", [256, D]); wpg_d = din("w_ple_gate", [D, D])
    cst_d = din("consts", [128, 6, 128])
    out_d = nc.dram_tensor("out", [NT, D], F32, kind="ExternalOutput").ap()

    x1_d = dscr("x1_s", [NT, D], F32)
    hgT_d = dscr("hgT_s", [8, 128, NT], BF16)
    hmT_d = dscr("hmT_s", [8, 128, NT], BF16)
    mgT_d = dscr("mgT_s", [16, 128, NT], BF16)
    xg_d = dscr("xg_s", [NSLOT, D], BF16)
    y_d = dscr("y_s", [NSLOT, D], F32)
    w1b_d = nc.dram_tensor("w1b_s", [32, D, 512], BF16, kind="Internal").ap()
    w3b_d = nc.dram_tensor("w3b_s", [32, D, 512], BF16, kind="Internal").ap()
    w2b_d = nc.dram_tensor("w2b_s", [32, 512, D], BF16, kind="Internal").ap()
    woutb_d = nc.dram_tensor("woutb_s", [D, D], BF16, kind="Internal").ap()
    wpgb_d = nc.dram_tensor("wpgb_s", [D, D], BF16, kind="Internal").ap()
    wpub_d = nc.dram_tensor("wpub_s", [256, D], BF16, kind="Internal").ap()
    dbg_d = {}

    def ddbg(name, shape, dt=F32):
        if dbg:
            dbg_d[name] = nc.dram_tensor("dbg_" + name, list(shape), dt, kind="ExternalOutput").ap()
            return dbg_d[name]
        return None

    with contextlib.ExitStack() as es0:
        k = KB(nc, es0)
        cstf = k.sb(es0, "cstf", [128, 6, 128], F32)
        identb = k.sb(es0, "identb", [128, 128], BF16)
        flag = k.sb(es0, "flag", [128, 1], F32)
        slots_all = k.sb(es0, "slots_all", [128, NCH * 2], I32)
        wts_all = k.sb(es0, "wts_all", [128, NCH, 2], F32)
        R_c = Res("consts"); R_C = Res("C"); R_Cb = Res("Cb"); R_sw = Res("slotw")
        k.dma("sp", cstf[:], cst_d[:], writes=[R_c], add=True)
        k.dma("sp", flag[:], flag_d[:], writes=[R_c], add=True)
        k.op("dve", lambda e: e.tensor_copy(out=identb[:], in_=cstf[:, 0, :]), reads=[R_c], writes=[R_c], add=True)
        mhalf = k.sb(es0, "mhalf", [128, 16], F32)
        k.op("dve", lambda e: e.memset(mhalf[:], -0.5), writes=[R_c], add=True)
        identf = cstf[:, 0, :]; U = cstf[:, 1, :]; ones = cstf[:, 2, :]; negmT = cstf[:, 3, :]
        Ustr = cstf[:, 4, :]; iota32 = cstf[:, 5, 0:32]

        PS = [k.ps(es0, f"ps{i}", [128, 512], F32) for i in range(8)]
        PSB = [PS[i][:].bitcast(BF16) for i in range(8)]
        RP = [Res(f"ps{i}") for i in range(8)]

        R_z = Res("z"); R_xg = Res("xg")
        bc_reg = nc.gpsimd.to_reg(NSLOT - 1)

        def rstd_from_ss(ss, rs, R_ss, R_rs, n):
            k.op("dve", lambda e: e.tensor_scalar(out=rs, in0=ss, scalar1=1.0 / n, scalar2=EPS,
                                                  op0=ALU.mult, op1=ALU.add), reads=[R_ss], writes=[R_rs])
            k.op("pool", lambda e: e.tensor_tensor(out=rs, in0=rs, in1=mhalf[:, 0:1], op=ALU.pow), reads=[R_rs, R_c], writes=[R_rs])

        def norm_phase(tiles, grow, R_grow, hnT, R_hn):
            with contextlib.ExitStack() as es:
                xb = [k.sb(es, f"n_x{i}", [128, D], F32) for i in range(2)]
                junk = k.sb(es, "n_junk", [128, D], BF16)
                xs = [k.sb(es, f"n_xs{i}", [128, D], BF16) for i in range(2)]
                ss = k.sb(es, "n_ss", [128, 2], F32)
                R_x = [Res(), Res()]; R_j = Res(); R_xs = [Res(), Res()]; R_s = Res(); R_r = Res()
                for ti, (src, col0, nuse, rkey) in enumerate(tiles):
                    b = ti % 2
                    k.dma("sp", xb[b][:], src, writes=[R_x[b]])
                    k.op("act", lambda e: e.activation(out=junk[:], in_=xb[b][:], func=AF.Square,
                                                       accum_out=ss[:, 0:1]), reads=[R_x[b]], writes=[R_j, R_s])
                    rstd_from_ss(ss[:, 0:1], ss[:, 1:2], R_s, R_r, D)
                    k.op("dve", lambda e: e.scalar_tensor_tensor(out=xs[b][:], in0=xb[b][:], scalar=ss[:, 1:2],
                                                                 in1=grow[:], op0=ALU.mult, op1=ALU.mult),
                         reads=[R_x[b], R_r, R_grow], writes=[R_xs[b]])
                    for hb in range(2):
                        pb = PSB[hb + 2 * b]
                        k.group("pe", [TR(pb[:, j * 128:(j + 1) * 128], xs[b][:, (hb * 8 + j) * 128:(hb * 8 + j + 1) * 128],
                                          identb[:]) for j in range(8)], reads=[R_xs[b], R_c], writes=[RP[hb + 2 * b]])
                        src3 = pb.rearrange("p (a t) -> p a t", t=128)[:, :, 128 - nuse:128]
                        dst3 = hnT[:, hb * 8:(hb + 1) * 8, col0:col0 + nuse]
                        eng = "act" if hb == 0 else "dve"
                        if eng == "act":
                            k.op("act", lambda e: e.copy(out=dst3, in_=src3), reads=[RP[hb + 2 * b]], writes=[R_hn[rkey]])
                        else:
                            k.op("dve", lambda e: e.tensor_copy(out=dst3, in_=src3), reads=[RP[hb + 2 * b]], writes=[R_hn[rkey]])
                k.barrier()

        def load_w_cast(dst, src_rows_cols, kchunks, R_dst, step=4, new_fill=True):
            v = src_rows_cols.rearrange("(k p) c -> p k c", p=128)
            if new_fill:
                k.start_fill(R_dst)
            for k0 in range(0, kchunks, step):
                k1 = min(kchunks, k0 + step)
                k.dma("pool", dst[:, k0:k1, :], v[:, k0:k1, :], writes=[R_dst], add=True)

        R_wb = Res("wb"); R_wbd = Res("wbd")
        conv_list = [(w1_d[e_], w1b_d[e_]) for e_ in range(32)] + [(w3_d[e_], w3b_d[e_]) for e_ in range(32)]
        conv_list = [x for pair in zip(conv_list[:32], conv_list[32:]) for x in pair] + [(w2_d[e_], w2b_d[e_]) for e_ in range(32)]
        dense_list = ([(wout_d[q * 512:(q + 1) * 512, :], woutb_d[q * 512:(q + 1) * 512, :]) for q in range(4)]
                      + [(wpg_d[q * 512:(q + 1) * 512, :], wpgb_d[q * 512:(q + 1) * 512, :]) for q in range(4)]
                      + [(wpu_d[:, :], wpub_d[:, :])])
        N_EARLY = 64
        conv_list = conv_list[:N_EARLY] + dense_list + conv_list[N_EARLY:]
        conv_pos = [0]

        def convert_some(n):
            for _ in range(n):
                if conv_pos[0] >= len(conv_list):
                    return
                src, dst = conv_list[conv_pos[0]]
                conv_pos[0] += 1
                k.dma("cv", dst.rearrange("(k p) c -> p k c", p=128), src.rearrange("(k p) c -> p k c", p=128),
                      writes=[R_wbd if N_EARLY < conv_pos[0] <= N_EARLY + 9 else R_wb], add=True)

        def row_bcast(es, name, src1d, n, R):
            t = k.sb(es, name, [128, n], F32)
            k.dma("sp", t[:], src1d.partition_broadcast(128), writes=[R], add=True)
            return t

        def gate_prep(es, hnT, R_hn, wif, R_wif, bgrow, R_bg):
            G = {}
            gsb = k.sb(es, "g_gsb", [128, NCH, 8], F32)
            lf = k.sb(es, "g_lf", [128, NCH, 4], F32)
            bcol = k.sb(es, "g_bcol", [128, NCH, 4], F32)
            biasc = k.sb(es, "g_biasc", [128, NCH, 4], F32)
            gcol = k.sb(es, "g_gcol", [128, NCH, 4], F32)
            wcol = k.sb(es, "g_wcol", [128, NCH, 4], F32)
            egcol = k.sb(es, "g_egcol", [128, NCH, 4], F32)
            R_g = Res("gates")
            for c in range(NCH):
                k.group("pe", [MM(PS[4][:, 0:8], hnT[:, kk, 3 + c * 128:3 + (c + 1) * 128], wif[:, kk, :], kk == 0, kk == KD - 1)
                               for kk in range(KD)], reads=[R_hn[c], R_wif], writes=[RP[4]])
                k.op("dve", lambda e: e.tensor_tensor(out=gsb[:, c, :], in0=PS[4][:, 0:8], in1=bgrow[:], op=ALU.add),
                     reads=[RP[4], R_bg], writes=[R_g])
            k.op("act", lambda e: e.activation(out=lf[:], in_=gsb[:, :, 4:8], func=AF.Exp, scale=-1.0), reads=[R_g], writes=[R_g])
            k.op("act", lambda e: e.activation(out=lf[:], in_=lf[:], func=AF.Ln, bias=1.0), reads=[R_g], writes=[R_g])
            k.op("dve", lambda e: e.tensor_scalar_mul(out=lf[:], in0=lf[:], scalar1=-1.0), reads=[R_g], writes=[R_g])
            lf2 = lf[:].rearrange("p c h -> p (c h)")
            k.op("pe", MM(PS[4][:, 0:64], U, lf2, True, True), reads=[R_g, R_c], writes=[RP[4]])
            k.op("dve", lambda e: e.tensor_copy(out=bcol[:].rearrange("p c h -> p (c h)"), in_=PS[4][:, 0:64]), reads=[RP[4]], writes=[R_g])
            k.op("pe", MM(PS[4][:, 0:64], ones, lf2, True, True), reads=[R_g, R_c], writes=[RP[4]])
            k.op("dve", lambda e: e.tensor_copy(out=gcol[:].rearrange("p c h -> p (c h)"), in_=PS[4][:, 0:64]), reads=[RP[4]], writes=[R_g])
            k.op("dve", lambda e: e.tensor_tensor(out=biasc[:], in0=gsb[:, :, 0:4], in1=bcol[:], op=ALU.subtract), reads=[R_g], writes=[R_g])
            k.op("dve", lambda e: e.tensor_tensor(out=wcol[:], in0=biasc[:], in1=gcol[:], op=ALU.add), reads=[R_g], writes=[R_g])
            k.op("act", lambda e: e.activation(out=wcol[:], in_=wcol[:], func=AF.Exp), reads=[R_g], writes=[R_g])
            k.op("act", lambda e: e.activation(out=egcol[:], in_=gcol[:], func=AF.Exp), reads=[R_g], writes=[R_g])
            G.update(lf=lf, biasc=biasc, wcol=wcol, egcol=egcol, R=R_g)
            return G

        def conv_proj(es, tag, wq, R_wq, colblk0, hnT, R_hn, outT, R_out, cw, cb, R_cw, scale, halo, CP):
            if "pc" not in CP:
                CP["pc"] = k.sb(es, tag + "_pc", [128, 3 + 512], F32)
                CP["acc"] = k.sb(es, tag + "_acc", [128, 512], F32)
                CP["R"] = (Res(), Res())
            pc = CP["pc"]; acc = CP["acc"]; R_pc, R_acc = CP["R"]
            for blk in range(2):
                cblk = colblk0 + blk
                if halo:
                    k.group("pe", [MM(PS[6][:, 0:3], wq[:, kk, blk * 128:(blk + 1) * 128], hnT[:, kk, 0:3], kk == 0, kk == KD - 1)
                                   for kk in range(KD)], reads=[R_hn["halo"], R_wq], writes=[RP[6]])
                    k.op("act", lambda e: e.copy(out=pc[:, 0:3], in_=PS[6][:, 0:3]), reads=[RP[6]], writes=[R_pc])
                else:
                    k.op("dve", lambda e: e.memset(pc[:, 0:3], 0.0), writes=[R_pc])
                for tt in range(4):
                    pb = 6 + (tt % 2)
                    k.group("pe", [MM(PS[pb][:], wq[:, kk, blk * 128:(blk + 1) * 128], hnT[:, kk, 3 + tt * 512:3 + (tt + 1) * 512],
                                      kk == 0, kk == KD - 1) for kk in range(KD)],
                            reads=[R_hn[tt * 4 + j] for j in range(4)] + [R_wq], writes=[RP[pb]])
                    k.op("act", lambda e: e.copy(out=pc[:, 3:515], in_=PS[pb][:]), reads=[RP[pb]], writes=[R_pc])
                    k.op("dve", lambda e: e.tensor_scalar_mul(out=acc[:], in0=pc[:, 0:512], scalar1=cw[:, cblk, 0:1]),
                         reads=[R_pc, R_cw], writes=[R_acc])
                    for j in range(1, 4):
                        k.op("dve", lambda e: e.scalar_tensor_tensor(out=acc[:], in0=pc[:, j:j + 512], scalar=cw[:, cblk, j:j + 1],
                                                                     in1=acc[:], op0=ALU.mult, op1=ALU.add),
                             reads=[R_pc, R_cw, R_acc], writes=[R_acc])
                    dst = outT[:, blk, tt * 512:(tt + 1) * 512]
                    k.op("act", lambda e: e.activation(out=dst, in_=acc[:], func=AF.Silu, bias=cb[:, cblk:cblk + 1]),
                         reads=[R_acc, R_cw], writes=[R_out])
                    if scale != 1.0:
                        k.op("dve", lambda e: e.tensor_scalar_mul(out=dst, in0=dst, scalar1=scale), reads=[R_out], writes=[R_out])
                    k.op("dve", lambda e: e.tensor_copy(out=pc[:, 0:3], in_=pc[:, 512:515]), reads=[R_pc], writes=[R_pc])
                    yield

        def state_update(T, h, c, G, kT, R_kT, vaug, R_va):
            pbT = PSB[5]
            k.group("pe", [TR(pbT[:, blk * 128:(blk + 1) * 128], kT[:, blk, c * 128:(c + 1) * 128], identb[:]) for blk in range(2)],
                    reads=[R_kT, R_c], writes=[RP[5]])
            k.op("act", lambda e: e.copy(out=T["ktok"][:], in_=pbT[:, 0:256]), reads=[RP[5]], writes=[T["R_ktok"]])
            k.op("dve", lambda e: e.tensor_scalar_mul(out=T["wv"][:], in0=vaug[:], scalar1=G["wcol"][:, c, h:h + 1]),
                 reads=[R_va, G["R"]], writes=[T["R_wv"]])
            for blk in range(2):
                k.op("pe", MM(PS[2 + blk][:, 0:257], T["ktok"][:, blk * 128:(blk + 1) * 128], T["wv"][:], True, True),
                     reads=[T["R_ktok"], T["R_wv"]], writes=[RP[2 + blk]])
                k.op("dve", lambda e: e.scalar_tensor_tensor(out=Cst[:, h, blk, :], in0=Cst[:, h, blk, :],
                                                             scalar=G["egcol"][:, c, h:h + 1], in1=PS[2 + blk][:, 0:257],
                                                             op0=ALU.mult, op1=ALU.add),
                     reads=[RP[2 + blk], G["R"], R_C], writes=[R_C])

        def vproj(hnT, R_hn, c, wv, R_wv, vaug, R_va):
            k.group("pe", [MM(PS[0][:, 0:256], hnT[:, kk, 3 + c * 128:3 + (c + 1) * 128], wv[:, kk, :], kk == 0, kk == KD - 1)
                           for kk in range(KD)], reads=[R_hn[c], R_wv], writes=[RP[0]])
            k.op("act", lambda e: e.copy(out=vaug[:, 0:256], in_=PS[0][:, 0:256]), reads=[RP[0]], writes=[R_va])

        def chunk_temps(es):
            T = {}
            T["ktok"] = k.sb(es, "t_ktok", [128, 256], BF16); T["R_ktok"] = Res()
            T["wv"] = k.sb(es, "t_wv", [128, 257], BF16); T["R_wv"] = Res()
            T["vaug"] = [k.sb(es, f"t_vaug{i}", [128, 257], BF16) for i in range(2)]
            T["R_va"] = [Res(), Res()]
            for i in range(2):
                k.op("dve", lambda e: e.memset(T["vaug"][i][:, 256:257], 1.0), writes=[T["R_va"][i]])
            return T


        def cols(c):
            return slice(3 + c * 128, 3 + (c + 1) * 128)

        def batch_V(hnT, R_hn, wv, R_wv, wo, R_wo, vaug_all, R_vac, sig_all, R_sigc):
            for c in range(NCH):
                pb = c % 2
                k.group("pe", [MM(PS[pb][:, 0:256], hnT[:, kk, cols(c)], wv[:, kk, :], kk == 0, kk == KD - 1) for kk in range(KD)],
                        reads=[R_hn[c], R_wv], writes=[RP[pb]])
                if wo is not None:
                    k.group("pe", [MM(PS[pb][:, 256:512], hnT[:, kk, cols(c)], wo[:, kk, :], kk == 0, kk == KD - 1) for kk in range(KD)],
                            reads=[R_hn[c], R_wo], writes=[RP[pb]], add=True)
                k.op("act", lambda e: e.copy(out=vaug_all[:, c, 0:256], in_=PS[pb][:, 0:256]), reads=[RP[pb]], writes=[R_vac[c]])
                if wo is not None:
                    k.op("act", lambda e: e.activation(out=sig_all[:, c, :], in_=PS[pb][:, 256:512], func=AF.Sigmoid), reads=[RP[pb]], writes=[R_sigc[c]])

        def batch_K(h, G, kT, R_kT, ktok_all, R_ktc, vaug_all, R_vac, wv_all, R_wvc):
            for c4 in range(4):
                bank = 6 + c4 % 2
                k.group("pe", [TR(PSB[bank][:, (j * 2 + blk) * 128:(j * 2 + blk + 1) * 128], kT[:, blk, (c4 * 4 + j) * 128:(c4 * 4 + j + 1) * 128], identb[:])
                               for j in range(4) for blk in range(2)], reads=[R_kT, R_c], writes=[RP[bank]])
                dst = ktok_all[:, c4 * 4:(c4 + 1) * 4, :]
                src = PSB[bank][:, 0:1024].rearrange("p (a b) -> p a b", b=256)
                if c4 % 2 == 0:
                    k.op("act", lambda e: e.copy(out=dst, in_=src), reads=[RP[bank]], writes=[R_ktc[c4]])
                else:
                    k.op("dve", lambda e: e.tensor_copy(out=dst, in_=src), reads=[RP[bank]], writes=[R_ktc[c4]])
            for c in range(NCH):
                k.op("dve", lambda e: e.tensor_scalar_mul(out=wv_all[:, c, 0:257], in0=vaug_all[:, c, 0:257], scalar1=G["wcol"][:, c, h:h + 1]),
                     reads=[R_vac[c], G["R"]], writes=[R_wvc[c]])

        def kv_mm(c, ktok_all, R_ktc, wv_all, R_wvc):
            for blk in range(2):
                bank = (c % 2) * 2 + blk
                k.op("pe", MM(PS[bank][:, 0:257], ktok_all[:, c, blk * 128:(blk + 1) * 128], wv_all[:, c, 0:257], True, True),
                     reads=[R_ktc[c // 4], R_wvc[c]], writes=[RP[bank]])

        def c_update(h, c, G, Cst, R_Ch):
            for blk in range(2):
                bank = (c % 2) * 2 + blk
                k.op("dve", lambda e: e.scalar_tensor_tensor(out=Cst[:, h, blk, :], in0=Cst[:, h, blk, :], scalar=G["egcol"][:, c, h:h + 1],
                                                             in1=PS[bank][:, 0:257], op0=ALU.mult, op1=ALU.add),
                     reads=[RP[bank], G["R"], R_Ch], writes=[R_Ch])

        with contextlib.ExitStack() as es1:
            hnT = k.sb(es1, "hnT", [128, KD, HT], BF16)
            Cst = k.sb(es1, "Cst", [128, 4, 2, 257], F32)
            k.op("dve", lambda e: e.memset(Cst[:], 0.0), writes=[R_C])
            R_hn = {c: Res(f"hn{c}") for c in range(NCH)}
            R_hn["halo"] = Res("hnhalo")
            wif = k.sb(es1, "wif", [128, KD, 8], BF16); R_wif = Res()
            load_w_cast(wif, w_in[:, C_IF:C_IF + 8], KD, R_wif, step=16)
            bgrow = row_bcast(es1, "bgrow", bgate_d, 8, R_c)
            cw = k.sb(es1, "cw", [128, 16, 4], F32); cb = k.sb(es1, "cb", [128, 16], F32); R_cw = Res()
            k.dma("sp", cw[:], convw_d[:], writes=[R_cw], add=True); k.dma("sp", cb[:], convb_d[:], writes=[R_cw], add=True)

            with contextlib.ExitStack() as es:
                grow = row_bcast(es, "gmixrow", gmix_d, D, R_c)
                zt = k.sb(es, "zt", [128, D], BF16)
                k.op("pool", lambda e: e.memset(zt[:], 0.0), writes=[R_z])
                for i in range(NSLOT // 128):
                    k.dma("sp", xg_d[i * 128:(i + 1) * 128, :], zt[:], reads=[R_z], writes=[R_xg], add=True)
                k.op("dve", lambda e: e.memset(hnT[:, :, 0:3], 0.0), writes=[R_hn["halo"]])
                norm_phase([(xp[i * 128:(i + 1) * 128, :], 3 + i * 128, 128, i) for i in range(NCH)], grow, R_c, hnT, R_hn)
            with contextlib.ExitStack() as es:
                G = gate_prep(es, hnT, R_hn, wif, R_wif, bgrow, R_c)
                CP = {}
                wk = k.sb(es, "p_wk", [128, KD, 256], BF16); wv = k.sb(es, "p_wv", [128, KD, 256], BF16)
                kT = k.sb(es, "p_kT", [128, 2, NT], BF16)
                vaug_all = k.sb(es, "p_vaug", [128, NCH, VW], BF16); wv_all = k.sb(es, "p_wvall", [128, NCH, VW], BF16)
                ktok_all = k.sb(es, "p_ktok", [128, NCH, 256], BF16)
                R_wk = Res(); R_wv = Res(); R_kT = Res()
                R_vac = [Res() for _ in range(NCH)]; R_wvc = [Res() for _ in range(NCH)]; R_ktc = [Res() for _ in range(4)]
                R_Ch = [Res() for _ in range(4)]
                k.op("dve", lambda e: e.memset(vaug_all[:, :, 256:257], 1.0), writes=R_vac)
                for h in range(4):
                    load_w_cast(wk, w_in[:, C_K + h * 256:C_K + (h + 1) * 256], KD, R_wk)
                    load_w_cast(wv, w_in[:, C_V + h * 256:C_V + (h + 1) * 256], KD, R_wv)
                    convert_some(6)
                    for _ in conv_proj(es, f"pk{h}", wk, R_wk, 8 + 2 * h, hnT, R_hn, kT, R_kT, cw, cb, R_cw, 1.0 / 16.0, halo=False, CP=CP):
                        pass
                    batch_V(hnT, R_hn, wv, R_wv, None, None, vaug_all, R_vac, None, None)
                    batch_K(h, G, kT, R_kT, ktok_all, R_ktc, vaug_all, R_vac, wv_all, R_wvc)
                    kv_mm(0, ktok_all, R_ktc, wv_all, R_wvc)
                    for c in range(NCH):
                        if c + 1 < NCH:
                            kv_mm(c + 1, ktok_all, R_ktc, wv_all, R_wvc)
                        c_update(h, c, G, Cst, R_Ch[h])
                k.op("dve", lambda e: e.tensor_scalar_mul(out=Cst[:].rearrange("p a b c -> p (a b c)"),
                                                          in0=Cst[:].rearrange("p a b c -> p (a b c)"), scalar1=flag[:, 0:1]),
                     reads=R_Ch + [R_c, R_C], writes=[R_C])
                k.barrier()
            if dbg:
                o = ddbg("Cpre", [128, 4 * 2 * 257])
                k.dma("sp", o, Cst[:].rearrange("p a b c -> p (a b c)"), reads=[R_C], writes=[Res()])

            with contextlib.ExitStack() as es:
                grow = row_bcast(es, "gmixrow2", gmix_d, D, R_c)
                tiles = [(xh[:, :], 0, 3, "halo")] + [(xm[i * 128:(i + 1) * 128, :], 3 + i * 128, 128, i) for i in range(NCH)]
                norm_phase(tiles, grow, R_c, hnT, R_hn)
            if dbg:
                o = ddbg("hnT", [128, KD * HT], BF16)
                k.dma("sp", o, hnT[:].rearrange("p a b -> p (a b)"), reads=list(R_hn.values()), writes=[Res()])

            R_hg = Res("hgT_d")
            with contextlib.ExitStack() as es:
                wu = k.sb(es, "g_wu", [128, KD, 1024], BF16); wvg = k.sb(es, "g_wv", [128, KD, 1024], BF16)
                R_wu = Res(); R_wvg = Res()
                load_w_cast(wu, w_in[:, C_U:C_U + 1024], KD, R_wu, step=2)
                load_w_cast(wvg, w_in[:, C_VG:C_VG + 1024], KD, R_wvg, step=2)
                convert_some(8)
                lngrow = row_bcast(es, "lngrow", lng_d, 1024, R_c)
                lnbrow = row_bcast(es, "lnbrow", lnb_d, 1024, R_c)
                bsrow = row_bcast(es, "bsrow", bs_d, 1024, R_c)
                guT = [k.sb(es, f"g_guT{i}", [128, 8, 512], BF16) for i in range(2)]; R_gu = [Res(), Res()]
                gv = [k.sb(es, f"g_gv{i}", [128, 1024], F32) for i in range(3)]; R_gv = [Res(), Res(), Res()]
                tmp = [k.sb(es, f"g_tmp{i}", [128, 1024], F32) for i in range(2)]; R_tmp = [Res(), Res()]
                vn = [k.sb(es, f"g_vn{i}", [128, 1024], BF16) for i in range(2)]; R_vn = [Res(), Res()]
                stt = k.sb(es, "g_st", [128, 2, 6], F32); mv = k.sb(es, "g_mv", [128, 4], F32); R_st = Res()
                hgs_ = k.sb(es, "g_hgs", [128, 8, 512], BF16); hgs = [hgs_, hgs_]; R_hgs_ = Res(); R_hgs = [R_hgs_, R_hgs_]

                wsn = tmp[0][:].rearrange("p (g t) -> p g t", t=128); wsT = k.sb(es, "g_wsT", [128, 8, 128], BF16)
                R_ws = Res()
                k.dma("sp", wsn, ws_d.rearrange("g t s -> t g s"), writes=[R_tmp[0]])
                for g in range(8):
                    k.op("pe", TR(PS[0][:, 0:128], wsn[:, g, :], identf), reads=[R_tmp[0], R_c], writes=[RP[0]])
                    k.op("dve", lambda e: e.tensor_tensor(out=wsT[:, g, :], in0=PS[0][:, 0:128], in1=U, op=ALU.mult),
                         reads=[RP[0], R_c], writes=[R_ws])
                def stageU(st):
                    for g in range(8):
                        pb = 6 + (g % 2)
                        k.group("pe", [MM(PS[pb][:], wu[:, kk, g * 128:(g + 1) * 128], hnT[:, kk, 3 + st * 512:3 + (st + 1) * 512],
                                          kk == 0, kk == KD - 1) for kk in range(KD)],
                                reads=[R_hn[st * 4 + j] for j in range(4)] + [R_wu], writes=[RP[pb]])
                        k.op("act", lambda e: e.activation(out=guT[st % 2][:, g, :], in_=PS[pb][:], func=AF.Gelu_apprx_tanh),
                             reads=[RP[pb]], writes=[R_gu[st % 2]])

                def stageV(c):
                    b = c % 3
                    for nb in range(2):
                        k.group("pe", [MM(PS[nb][:], hnT[:, kk, 3 + c * 128:3 + (c + 1) * 128], wvg[:, kk, nb * 512:(nb + 1) * 512],
                                          kk == 0, kk == KD - 1) for kk in range(KD)], reads=[R_hn[c], R_wvg], writes=[RP[nb]])
                        k.op("act", lambda e: e.activation(out=gv[b][:, nb * 512:(nb + 1) * 512], in_=PS[nb][:], func=AF.Gelu_apprx_tanh),
                             reads=[RP[nb]], writes=[R_gv[b]])

                def stageM(c):
                    b = c % 2
                    b3 = c % 3
                    st = c // 4; cc = c % 4
                    for nb in range(2):
                        k.op("dve", lambda e: e.bn_stats(out=stt[:, nb, :], in_=gv[b3][:, nb * 512:(nb + 1) * 512]), reads=[R_gv[b3]], writes=[R_st])
                    k.op("dve", lambda e: e.bn_aggr(out=mv[:, 0:2], in_=stt[:].rearrange("p a b -> p (a b)")), reads=[R_st], writes=[R_st])
                    k.op("dve", lambda e: e.tensor_scalar_add(out=mv[:, 2:3], in0=mv[:, 1:2], scalar1=EPS), reads=[R_st], writes=[R_st])
                    k.op("pool", lambda e: e.tensor_tensor(out=mv[:, 2:3], in0=mv[:, 2:3], in1=mhalf[:, 0:1], op=ALU.pow), reads=[R_st, R_c], writes=[R_st])
                    k.op("dve", lambda e: e.tensor_scalar(out=tmp[b][:], in0=gv[b3][:], scalar1=mv[:, 0:1], scalar2=mv[:, 2:3],
                                                          op0=ALU.subtract, op1=ALU.mult), reads=[R_gv[b3], R_st], writes=[R_tmp[b]])
                    k.op("pool", lambda e: e.tensor_tensor(out=tmp[b][:], in0=tmp[b][:], in1=lngrow[:], op=ALU.mult), reads=[R_tmp[b], R_c], writes=[R_tmp[b]])
                    k.op("pool", lambda e: e.tensor_tensor(out=vn[b][:], in0=tmp[b][:], in1=lnbrow[:], op=ALU.add), reads=[R_tmp[b], R_c], writes=[R_vn[b]])
                    for g in range(8):
                        pb = 2 + g // 4
                        k.op("pe", MM(PS[pb][:, (g % 4) * 128:(g % 4 + 1) * 128], vn[b][:, g * 128:(g + 1) * 128], wsT[:, g, :], True, True),
                             reads=[R_vn[b], R_ws], writes=[RP[pb]])
                    for hb in range(2):
                        k.op("dve", lambda e: e.tensor_tensor(out=tmp[b][:, hb * 512:(hb + 1) * 512], in0=PS[2 + hb][:],
                                                              in1=bsrow[:, hb * 512:(hb + 1) * 512], op=ALU.add),
                             reads=[RP[2 + hb], R_c], writes=[R_tmp[b]])
                    sb_ = st % 2
                    k.op("dve", lambda e: e.tensor_tensor(out=hgs[sb_][:, :, cc * 128:(cc + 1) * 128],
                                                          in0=tmp[b][:].rearrange("p (g t) -> p g t", t=128),
                                                          in1=guT[sb_][:, :, cc * 128:(cc + 1) * 128], op=ALU.mult),
                         reads=[R_tmp[b], R_gu[sb_]], writes=[R_hgs[sb_]])
                    if cc == 3:
                        k.dma("sp", hgT_d[:, :, st * 512:(st + 1) * 512].rearrange("g p t -> p g t"), hgs[sb_][:],
                              reads=[R_hgs[sb_]], writes=[R_hg], add=True)

                stageU(0)
                stageV(0)
                stageV(1)
                for c in range(NCH):
                    if c % 4 == 1 and c // 4 + 1 < 4:
                        stageU(c // 4 + 1)
                    if c + 2 < NCH:
                        stageV(c + 2)
                    stageM(c)
                k.barrier()

            if stop_after == "gmlp":
                k.barrier()
                return nc, dbg_d

            R_hmd = Res("hmT_d")
            with contextlib.ExitStack() as es:
                G = gate_prep(es, hnT, R_hn, wif, R_wif, bgrow, R_c)
                CP = {}
                gnrow = row_bcast(es, "gnrow", gnm_d, 1024, R_c)
                scrA = k.sb(es, "m_scrA", [128, 2 * KD * 256], BF16); R_scrA = Res()
                wq = k.sb(es, "m_wq", [128, KD, 256], BF16); wk = k.sb(es, "m_wk", [128, KD, 256], BF16); R_wq = Res(); R_wk = Res()
                ktok_all = scrA[:, 0:NCH * 256].rearrange("p (a b) -> p a b", b=256)
                EB = scrA[:, 4096:4096 + 2048]
                DT = scrA[:, 6144:6144 + 2048].rearrange("p (a b) -> p a b", b=128)
                wv = k.sb(es, "m_wv", [128, KD, 256], BF16); wo = k.sb(es, "m_wo", [128, KD, 256], BF16)
                qT = k.sb(es, "m_qT", [128, 2, NT], BF16); kT = k.sb(es, "m_kT", [128, 2, NT], BF16)
                vaug_all = k.sb(es, "m_vaug", [128, NCH, VW], BF16); wv_all = k.sb(es, "m_wvall", [128, NCH, VW], BF16)
                sig_all = k.sb(es, "m_sig", [128, NCH, 256], BF16)
                num_all = k.sb(es, "m_num", [128, NCH, 260], F32)
                Ulf_ = k.sb(es, "m_Ulf", [128, 4, 128], F32); Ulf = [Ulf_, Ulf_]
                argm_ = k.sb(es, "m_argm", [128, 4, 128], F32); argm = [argm_, argm_]
                Cb2 = k.sb(es, "m_Cb2", [128, 2, 2, VW], BF16)
                pmx = k.sb(es, "m_pm", [128, 4, NCH], F32); st_all = k.sb(es, "m_stall", [128, NCH, 6], F32); mv_all = k.sb(es, "m_mvall", [128, NCH, 2], F32)
                hmTh = k.sb(es, "m_hmTh", [128, 2, NT], BF16)
                R_wv = Res(); R_wo = Res(); R_qT = Res(); R_kT = Res()
                R_vac = [Res() for _ in range(NCH)]; R_wvc = [Res() for _ in range(NCH)]; R_sigc = [Res() for _ in range(NCH)]
                R_numc = [Res() for _ in range(NCH)]; R_stc = [Res() for _ in range(NCH)]
                R_Ulf_ = Res(); R_Ulf = [R_Ulf_, R_Ulf_]; R_argm_ = Res(); R_argm = [R_argm_, R_argm_]; R_Cb2 = [Res(), Res()]; R_pm = Res(); R_hmTh = Res(); R_Ch = Res()
                R_ktc = [R_scrA] * 4
                k.op("dve", lambda e: e.memset(vaug_all[:, :, 256:257], 1.0), writes=R_vac)
                def loads_qk(h):
                    load_w_cast(wq, w_in[:, C_Q + h * 256:C_Q + (h + 1) * 256], KD, R_wq)
                    load_w_cast(wk, w_in[:, C_K + h * 256:C_K + (h + 1) * 256], KD, R_wk)

                def loads_vo(h):
                    load_w_cast(wv, w_in[:, C_V + h * 256:C_V + (h + 1) * 256], KD, R_wv)
                    load_w_cast(wo, w_in[:, C_O + h * 256:C_O + (h + 1) * 256], KD, R_wo)
                    convert_some(8)

                def conv_qk(h):
                    yield from conv_proj(es, f"mq{h}", wq, R_wq, 2 * h, hnT, R_hn, qT, R_qT, cw, cb, R_cw, 1.0, halo=True, CP=CP)
                    yield from conv_proj(es, f"mk{h}", wk, R_wk, 8 + 2 * h, hnT, R_hn, kT, R_kT, cw, cb, R_cw, 1.0 / 16.0, halo=True, CP=CP)

                def mid(h):
                    batch_V(hnT, R_hn, wv, R_wv, wo, R_wo, vaug_all, R_vac, sig_all, R_sigc)
                    k.start_fill(R_scrA)
                    for c4 in range(4):
                        ub = c4 % 2
                        for j in range(4):
                            c = c4 * 4 + j
                            k.op("dve", lambda e: e.tensor_scalar_mul(out=Ulf[ub][:, j, :], in0=U, scalar1=G["lf"][:, c, h:h + 1]),
                                 reads=[R_c, G["R"]], writes=[R_Ulf[ub]], add=(j > 0))
                        k.op("pe", MM(PS[2 + ub][:], ones, Ulf[ub][:].rearrange("p a b -> p (a b)"), True, True), reads=[R_Ulf[ub], R_c], writes=[RP[2 + ub]])
                        for j in range(4):
                            c = c4 * 4 + j
                            k.op("dve", lambda e: e.scalar_tensor_tensor(out=argm[ub][:, j, :], in0=PS[2 + ub][:, j * 128:(j + 1) * 128],
                                                                         scalar=G["biasc"][:, c, h:h + 1], in1=negmT, op0=ALU.add, op1=ALU.add),
                                 reads=[RP[2 + ub], G["R"], R_c], writes=[R_argm[ub]], add=(j > 0))
                        k.op("act", lambda e: e.activation(out=DT[:, c4 * 4:(c4 + 1) * 4, :], in_=argm[ub][:], func=AF.Exp),
                             reads=[R_argm[ub]], writes=[R_scrA], add=True)
                        k.op("act", lambda e: e.activation(out=EB[:, c4 * 512:(c4 + 1) * 512], in_=PS[2 + ub][:], func=AF.Exp),
                             reads=[RP[2 + ub]], writes=[R_scrA], add=True)
                    for c4 in range(4):
                        bank = 4 + c4 % 2
                        for j in range(4):
                            cs = slice((c4 * 4 + j) * 128, (c4 * 4 + j + 1) * 128)
                            k.group("pe", [MM(PS[bank][:, j * 128:(j + 1) * 128], kT[:, blk, cs], qT[:, blk, cs], blk == 0, blk == 1) for blk in range(2)],
                                    reads=[R_kT, R_qT], writes=[RP[bank]], add=(j > 0))
                        dtv = DT[:, c4 * 4:(c4 + 1) * 4, :]
                        k.op("dve", lambda e: e.tensor_tensor(out=dtv, in0=PS[bank][:].rearrange("p (a b) -> p a b", b=128), in1=dtv, op=ALU.mult),
                             reads=[RP[bank], R_scrA], writes=[R_scrA], add=True)
                    for c4 in range(4):
                        bank = 6 + c4 % 2
                        k.group("pe", [TR(PSB[bank][:, (j * 2 + blk) * 128:(j * 2 + blk + 1) * 128], kT[:, blk, (c4 * 4 + j) * 128:(c4 * 4 + j + 1) * 128], identb[:])
                                       for j in range(4) for blk in range(2)], reads=[R_kT, R_c], writes=[RP[bank]])
                        dst = ktok_all[:, c4 * 4:(c4 + 1) * 4, :]
                        src = PSB[bank][:, 0:1024].rearrange("p (a b) -> p a b", b=256)
                        if c4 % 2 == 0:
                            k.op("act", lambda e: e.copy(out=dst, in_=src), reads=[RP[bank]], writes=[R_scrA], add=True)
                        else:
                            k.op("dve", lambda e: e.tensor_copy(out=dst, in_=src), reads=[RP[bank]], writes=[R_scrA], add=True)
                    for c in range(NCH - 1):
                        k.op("dve", lambda e: e.tensor_scalar_mul(out=wv_all[:, c, 0:257], in0=vaug_all[:, c, 0:257], scalar1=G["wcol"][:, c, h:h + 1]),
                             reads=[R_vac[c], G["R"]], writes=[R_wvc[c]])
                    for blk in range(2):
                        k.op("dve", lambda e: e.tensor_tensor(out=qT[:, blk, :], in0=qT[:, blk, :], in1=EB, op=ALU.mult),
                             reads=[R_qT, R_scrA], writes=[R_qT])
                    k.op("act", lambda e: e.copy(out=Cb2[:, 0, :, 0:257], in_=Cst[:, h, :, :]), reads=[R_C, R_Ch], writes=[R_Cb2[0]])
                    kv_mm(0, ktok_all, R_ktc, wv_all, R_wvc)
                    for c in range(NCH):
                        cs = slice(c * 128, (c + 1) * 128)
                        par = c % 2
                        k.group("pe", [MM(PS[4 + par][:, 0:257], DT[:, c, :], vaug_all[:, c, 0:257], True, False),
                                       MM(PS[4 + par][:, 0:257], qT[:, 0, cs], Cb2[:, par, 0, 0:257], False, False),
                                       MM(PS[4 + par][:, 0:257], qT[:, 1, cs], Cb2[:, par, 1, 0:257], False, True)],
                                reads=[R_scrA, R_vac[c], R_qT, R_Cb2[par]], writes=[RP[4 + par]])
                        k.op("act", lambda e: e.copy(out=num_all[:, c, 0:257], in_=PS[4 + par][:, 0:257]), reads=[RP[4 + par]], writes=[R_numc[c]])
                        if c + 1 < NCH - 1:
                            kv_mm(c + 1, ktok_all, R_ktc, wv_all, R_wvc)
                        if c < NCH - 1:
                            c_update(h, c, G, Cst, R_Ch)
                            k.op("act", lambda e: e.copy(out=Cb2[:, 1 - par, :, 0:257], in_=Cst[:, h, :, :]), reads=[R_Ch], writes=[R_Cb2[1 - par]])

                def post(h):
                    den = num_all[:, :, 256]
                    k.op("dve", lambda e: e.scalar_tensor_tensor(out=pmx[:, 0, :], in0=den, scalar=-1.0, in1=den, op0=ALU.mult, op1=ALU.max),
                         reads=R_numc, writes=[R_pm])
                    k.op("dve", lambda e: e.tensor_scalar_max(out=pmx[:, 0, :], in0=pmx[:, 0, :], scalar1=1.0), reads=[R_pm], writes=[R_pm])
                    k.op("dve", lambda e: e.reciprocal(out=pmx[:, 1, :], in_=pmx[:, 0, :]), reads=[R_pm], writes=[R_pm])
                    yield
                    for c in range(NCH):
                        hv = num_all[:, c, 0:256]
                        k.op("act", lambda e: e.activation(out=hv, in_=hv, func=AF.Copy, scale=pmx[:, 1, c:c + 1]), reads=[R_numc[c], R_pm], writes=[R_numc[c]])
                        k.op("dve", lambda e: e.bn_stats(out=st_all[:, c, :], in_=hv), reads=[R_numc[c]], writes=[R_stc[c]])
                        k.op("dve", lambda e: e.bn_aggr(out=mv_all[:, c, :], in_=st_all[:, c, :]), reads=[R_stc[c]], writes=[R_stc[c]])
                        yield
                    k.op("dve", lambda e: e.tensor_scalar_add(out=pmx[:, 2, :], in0=mv_all[:, :, 1], scalar1=EPS), reads=R_stc + [R_pm], writes=[R_pm])
                    k.op("pool", lambda e: e.tensor_tensor(out=pmx[:, 2, :], in0=pmx[:, 2, :], in1=mhalf[:, 0:NCH], op=ALU.pow), reads=[R_pm, R_c], writes=[R_pm])
                    yield
                    k.start_fill(R_hmTh)
                    for c in range(NCH):
                        hv = num_all[:, c, 0:256]
                        k.op("dve", lambda e: e.tensor_scalar(out=hv, in0=hv, scalar1=mv_all[:, c, 0:1], scalar2=pmx[:, 2, c:c + 1],
                                                              op0=ALU.subtract, op1=ALU.mult), reads=[R_numc[c], R_stc[c], R_pm], writes=[R_numc[c]])
                        k.op("pool", lambda e: e.tensor_tensor(out=hv, in0=hv, in1=gnrow[:, h * 256:(h + 1) * 256], op=ALU.mult),
                             reads=[R_numc[c], R_c], writes=[R_numc[c]])
                        k.op("dve", lambda e: e.tensor_tensor(out=sig_all[:, c, :], in0=hv, in1=sig_all[:, c, :], op=ALU.mult),
                             reads=[R_numc[c], R_sigc[c]], writes=[R_sigc[c]])
                        bank = 4 + c % 2
                        cs = slice(c * 128, (c + 1) * 128)
                        k.group("pe", [TR(PSB[bank][:, blk * 128:(blk + 1) * 128], sig_all[:, c, blk * 128:(blk + 1) * 128], identb[:]) for blk in range(2)],
                                reads=[R_sigc[c], R_c], writes=[RP[bank]])
                        if c % 2 == 0:
                            k.op("act", lambda e: e.copy(out=hmTh[:, 0:2, cs], in_=PSB[bank][:, 0:256].rearrange("p (a t) -> p a t", t=128)),
                                 reads=[RP[bank]], writes=[R_hmTh], add=True)
                        else:
                            k.op("dve", lambda e: e.tensor_copy(out=hmTh[:, 0:2, cs], in_=PSB[bank][:, 0:256].rearrange("p (a t) -> p a t", t=128)),
                                 reads=[RP[bank]], writes=[R_hmTh], add=True)
                        yield
                    for blk in range(2):
                        k.dma("sp", hmT_d[2 * h + blk, :, :], hmTh[:, blk, :], reads=[R_hmTh], writes=[R_hmd], add=True)

                def adv1(g):
                    try:
                        next(g)
                        return True
                    except StopIteration:
                        return False

                loads_qk(0)
                loads_vo(0)
                for _ in conv_qk(0):
                    pass
                for h in range(4):
                    if h + 1 < 4:
                        loads_qk(h + 1)
                    mid(h)
                    gp = post(h)
                    if h + 1 < 4:
                        loads_vo(h + 1)
                        for _ in conv_qk(h + 1):
                            adv1(gp)
                    while adv1(gp):
                        pass
                k.barrier()
            if stop_after == "mlstm":
                k.barrier()
                return nc, dbg_d

            R_mg = Res("mgT_d")
            with contextlib.ExitStack() as es:
                hgT = k.sb(es, "t_hgT", [128, 8, NT], BF16); R_hgl = Res()
                for g in range(8):
                    k.dma("sp", hgT[:, g, :], hgT_d[g, :, :], reads=[R_hg], writes=[R_hgl], add=True)
                hmT = k.sb(es, "t_hmT", [128, 8, NT], BF16); R_hm = Res()
                for g in range(8):
                    k.dma("sp", hmT[:, g, :], hmT_d[g, :, :], reads=[R_hmd], writes=[R_hm], add=True)
                GW = 256
                wbm = [k.sb(es, f"t_wbm{i}", [128, 8, GW], BF16) for i in range(2)]
                wbg = [k.sb(es, f"t_wbg{i}", [128, 8, GW], BF16) for i in range(2)]
                wgm = [k.sb(es, f"t_wgm{i}", [128, KD, GW], BF16) for i in range(2)]
                wgg = [k.sb(es, f"t_wgg{i}", [128, KD, GW], BF16) for i in range(2)]
                R_w = [Res(), Res()]
                sgA = k.sb(es, "t_sgA", [128, 512], F32); sgD = k.sb(es, "t_sgD", [128, 512], F32)
                m1 = k.sb(es, "t_m1", [128, 512], F32); m2 = k.sb(es, "t_m2", [128, 512], F32)
                mst = [k.sb(es, f"t_mst{i}", [128, 512], BF16) for i in range(2)]
                R_sA = Res(); R_sD = Res(); R_m1 = Res(); R_m2 = Res(); R_ms = [Res(), Res()]

                def load_group(gi):
                    b = gi % 2
                    c0 = gi * GW
                    load_w_cast(wbm[b], wbm_d[:, c0:c0 + GW], 8, R_w[b], step=8)
                    load_w_cast(wbg[b], wbg_d[:, c0:c0 + GW], 8, R_w[b], step=8, new_fill=False)
                    load_w_cast(wgm[b], w_in[:, C_GM + c0:C_GM + c0 + GW], KD, R_w[b], step=8, new_fill=False)
                    load_w_cast(wgg[b], w_in[:, C_GG + c0:C_GG + c0 + GW], KD, R_w[b], step=8, new_fill=False)
                    convert_some(6)
                NG = D // GW
                load_group(0)
                it = 0
                for gi in range(NG):
                    if gi + 1 < NG:
                        load_group(gi + 1)
                    b = gi % 2
                    for jj in range(GW // 128):
                        j = gi * (GW // 128) + jj
                        js = slice(jj * 128, (jj + 1) * 128)
                        for tt in range(4):
                            ts_ = slice(tt * 512, (tt + 1) * 512)
                            hs_ = slice(3 + tt * 512, 3 + (tt + 1) * 512)
                            pa = 4 * (it % 2)
                            hn_reads = [R_hn[tt * 4 + q] for q in range(4)]
                            k.group("pe", [MM(PS[pa][:], wbm[b][:, kk, js], hmT[:, kk, ts_], kk == 0, kk == 7) for kk in range(8)],
                                    reads=[R_w[b], R_hm], writes=[RP[pa]])
                            k.group("pe", [MM(PS[pa + 1][:], wgm[b][:, kk, js], hnT[:, kk, hs_], kk == 0, kk == KD - 1) for kk in range(KD)],
                                    reads=[R_w[b]] + hn_reads, writes=[RP[pa + 1]])
                            k.group("pe", [MM(PS[pa + 2][:], wbg[b][:, kk, js], hgT[:, kk, ts_], kk == 0, kk == 7) for kk in range(8)],
                                    reads=[R_w[b], R_hgl], writes=[RP[pa + 2]])
                            k.group("pe", [MM(PS[pa + 3][:], wgg[b][:, kk, js], hnT[:, kk, hs_], kk == 0, kk == KD - 1) for kk in range(KD)],
                                    reads=[R_w[b]] + hn_reads, writes=[RP[pa + 3]])
                            k.op("act", lambda e: e.activation(out=sgA[:], in_=PS[pa + 1][:], func=AF.Sigmoid), reads=[RP[pa + 1]], writes=[R_sA])
                            k.op("act", lambda e: e.activation(out=sgD[:], in_=PS[pa + 3][:], func=AF.Sigmoid), reads=[RP[pa + 3]], writes=[R_sD])
                            k.op("dve", lambda e: e.tensor_tensor(out=m1[:], in0=PS[pa][:], in1=sgA[:], op=ALU.mult), reads=[RP[pa], R_sA], writes=[R_m1])
                            k.op("dve", lambda e: e.tensor_tensor(out=m2[:], in0=PS[pa + 2][:], in1=sgD[:], op=ALU.mult), reads=[RP[pa + 2], R_sD], writes=[R_m2])
                            mb = it % 2
                            k.op("pool", lambda e: e.tensor_tensor(out=mst[mb][:], in0=m1[:], in1=m2[:], op=ALU.add), reads=[R_m1, R_m2], writes=[R_ms[mb]])
                            k.dma("sp", mgT_d[j, :, ts_], mst[mb][:], reads=[R_ms[mb]], writes=[R_mg], add=True)
                            it += 1
                k.barrier()
        if stop_after == "tail1":
            k.barrier()
            return nc, dbg_d

        R_x1 = Res("x1_d")
        with contextlib.ExitStack() as es:
            mgT = k.sb(es, "u_mgT", [128, KD, NT], BF16); R_mgl = Res()
            for j in range(KD):
                k.dma("sp", mgT[:, j, :], mgT_d[j, :, :], reads=[R_mg], writes=[R_mgl], add=True)
            wout = k.sb(es, "u_wout", [128, KD, D], BF16); R_wo2 = Res()
            k.start_fill(R_wo2)
            wv_ = woutb_d.rearrange("(k p) c -> p k c", p=128)
            for k0 in range(0, KD, 2):
                k.dma("act" if (k0 // 2) % 2 else "sp", wout[:, k0:k0 + 2, :], wv_[:, k0:k0 + 2, :], reads=[R_wbd], writes=[R_wo2], add=True)
            gfrow = row_bcast(es, "gffnrow", gffn_d, D, R_c)
            wr = k.sb(es, "u_wr", [128, KD, 36], F32); brrow = row_bcast(es, "brrow", br_d, 36, R_c)
            k.dma("sp", wr[:], wr_d.rearrange("(k p) c -> p k c", p=128), writes=[R_c], add=True)
            x1 = [k.sb(es, f"u_x1{i}", [128, D], F32) for i in range(2)]; R_x1t = [Res(), Res()]
            hn2 = [k.sb(es, f"u_hn2{i}", [128, D], F32) for i in range(3)]; R_h2 = [Res(), Res(), Res()]
            hn2b = [k.sb(es, f"u_hn2b{i}", [128, D], BF16) for i in range(3)]; R_h2b = [Res(), Res(), Res()]
            h2T = k.sb(es, "u_h2T", [128, KD, 128], F32); R_h2T = Res()
            ss = k.sb(es, "u_ss", [128, 2], F32); R_s = Res(); R_r = Res()
            L = k.sb(es, "u_L", [128, 36], F32); R_L = Res()
            rt = k.sb(es, "u_rt", [128, 16], F32); R_rt = Res()
            oh = k.sb(es, "u_oh", [128, 4], F32)
            msk = k.sb(es, "u_msk", [128, 32], F32); sel = k.sb(es, "u_sel", [128, 32], F32)
            oh1 = k.sb(es, "u_oh1", [128, 32], F32); oh2 = k.sb(es, "u_oh2", [128, 32], F32)
            m2_ = k.sb(es, "u_m2", [128, 32], F32); t32 = k.sb(es, "u_t32", [128, 32], F32)
            base = k.sb(es, "u_base", [128, 32], F32); spos = k.sb(es, "u_spos", [128, 32], F32)
            slf = k.sb(es, "u_slf", [128, 2], F32)
            sli = [k.sb(es, f"u_sli{i}", [128, 2], I32) for i in range(3)]; R_sli = [Res(), Res(), Res()]
            R_rr = Res("route")
            k.op("dve", lambda e: e.memset(base[:], 0.0), writes=[R_rr])
            def stageA(i):
                b = i % 2
                b3 = i % 3
                rows = slice(i * 128, (i + 1) * 128)
                k.dma("sp", x1[b][:], xm[rows, :], writes=[R_x1t[b]])
                for nb in range(4):
                    k.group("pe", [MM(PS[nb][:], mgT[:, kk, rows], wout[:, kk, nb * 512:(nb + 1) * 512], kk == 0, kk == KD - 1) for kk in range(KD)],
                            reads=[R_mgl, R_wo2], writes=[RP[nb]])
                    k.op("dve", lambda e: e.tensor_tensor(out=x1[b][:, nb * 512:(nb + 1) * 512], in0=PS[nb][:], in1=x1[b][:, nb * 512:(nb + 1) * 512], op=ALU.add),
                         reads=[RP[nb], R_x1t[b]], writes=[R_x1t[b]], add=True)
                    yield
                k.dma("sp", x1_d[rows, :], x1[b][:], reads=[R_x1t[b]], writes=[R_x1], add=True)
                k.op("act", lambda e: e.activation(out=hn2b[b3][:], in_=x1[b][:], func=AF.Square, accum_out=ss[:, 0:1]), reads=[R_x1t[b]], writes=[R_h2b[b3], R_s])
                rstd_from_ss(ss[:, 0:1], ss[:, 1:2], R_s, R_r, D)
                k.op("dve", lambda e: e.scalar_tensor_tensor(out=hn2[b3][:], in0=x1[b][:], scalar=ss[:, 1:2], in1=gfrow[:], op0=ALU.mult, op1=ALU.mult),
                     reads=[R_x1t[b], R_r, R_c], writes=[R_h2[b3]])
                k.op("act", lambda e: e.copy(out=hn2b[b3][:], in_=hn2[b3][:]), reads=[R_h2[b3]], writes=[R_h2b[b3]])

            def stageB(i):
                b = i % 3
                for q4 in range(4):
                    pb = 4 + (q4 % 2)
                    k.group("pe", [TR(PS[pb][:, j * 128:(j + 1) * 128], hn2[b][:, (q4 * 4 + j) * 128:(q4 * 4 + j + 1) * 128], identf) for j in range(4)],
                            reads=[R_h2[b], R_c], writes=[RP[pb]])
                    k.op("act", lambda e: e.copy(out=h2T[:, q4 * 4:(q4 + 1) * 4, :], in_=PS[pb][:].rearrange("p (a t) -> p a t", t=128)),
                         reads=[RP[pb]], writes=[R_h2T])
                yield
                k.group("pe", [MM(PS[6][:, 0:36], h2T[:, kk, :], wr[:, kk, :], kk == 0, kk == KD - 1) for kk in range(KD)],
                        reads=[R_h2T, R_c], writes=[RP[6]])
                k.op("dve", lambda e: e.tensor_tensor(out=L[:], in0=PS[6][:, 0:36], in1=brrow[:], op=ALU.add), reads=[RP[6], R_c], writes=[R_rr])
                yield

                def dv(fn):
                    k.op("dve", fn, reads=[R_rr, R_c], writes=[R_rr])

                def ac(fn):
                    k.op("act", fn, reads=[R_rr, R_c], writes=[R_rr])
                dv(lambda e: e.reduce_max(out=rt[:, 0:1], in_=L[:, 0:4], axis=mybir.AxisListType.X))
                dv(lambda e: e.tensor_scalar(out=oh[:], in0=L[:, 0:4], scalar1=rt[:, 0:1], scalar2=None, op0=ALU.is_equal))
                dv(lambda e: e.tensor_scalar_mul(out=rt[:, 1:2], in0=rt[:, 0:1], scalar1=-1.0))
                ac(lambda e: e.activation(out=t32[:, 0:4], in_=L[:, 0:4], func=AF.Exp, bias=rt[:, 1:2], accum_out=rt[:, 2:3]))
                dv(lambda e: e.reciprocal(out=rt[:, 3:4], in_=rt[:, 2:3]))
                dv(lambda e: e.tensor_scalar(out=oh[:], in0=oh[:], scalar1=1e4, scalar2=-1e4, op0=ALU.mult, op1=ALU.add))
                for gq in range(4):
                    dv(lambda e, gq=gq: e.tensor_scalar(out=msk[:, gq * 8:(gq + 1) * 8], in0=L[:, 4 + gq * 8:4 + (gq + 1) * 8],
                                                        scalar1=oh[:, gq:gq + 1], scalar2=None, op0=ALU.add))
                yield
                dv(lambda e: e.reduce_max(out=rt[:, 4:5], in_=msk[:], axis=mybir.AxisListType.X))
                dv(lambda e: e.tensor_scalar(out=oh1[:], in0=msk[:], scalar1=rt[:, 4:5], scalar2=None, op0=ALU.is_equal))
                dv(lambda e: e.scalar_tensor_tensor(out=m2_[:], in0=oh1[:], scalar=-3e4, in1=msk[:], op0=ALU.mult, op1=ALU.add))
                dv(lambda e: e.reduce_max(out=rt[:, 5:6], in_=m2_[:], axis=mybir.AxisListType.X))
                dv(lambda e: e.tensor_scalar(out=oh2[:], in0=m2_[:], scalar1=rt[:, 5:6], scalar2=None, op0=ALU.is_equal))
                dv(lambda e: e.tensor_tensor(out=sel[:], in0=oh1[:], in1=oh2[:], op=ALU.add))
                dv(lambda e: e.tensor_tensor(out=rt[:, 6:7], in0=rt[:, 5:6], in1=rt[:, 4:5], op=ALU.subtract))
                ac(lambda e: e.activation(out=rt[:, 7:8], in_=rt[:, 6:7], func=AF.Exp))
                dv(lambda e: e.tensor_scalar_add(out=rt[:, 8:9], in0=rt[:, 7:8], scalar1=1.0))
                dv(lambda e: e.reciprocal(out=rt[:, 8:9], in_=rt[:, 8:9]))
                k.op("dve", lambda e: e.tensor_tensor(out=wts_all[:, i, 0:1], in0=rt[:, 8:9], in1=rt[:, 3:4], op=ALU.mult), reads=[R_rr], writes=[R_rr, R_sw])
                k.op("dve", lambda e: e.tensor_tensor(out=wts_all[:, i, 1:2], in0=wts_all[:, i, 0:1], in1=rt[:, 7:8], op=ALU.mult), reads=[R_rr], writes=[R_rr, R_sw])
                yield
                k.op("pe", MM(PS[7][:, 0:32], Ustr, sel[:], True, True), reads=[R_rr, R_c], writes=[RP[7]])
                k.op("pe", MM(PS[7][:, 32:64], ones, sel[:], True, True), reads=[R_rr, R_c], writes=[RP[7]])
                k.op("dve", lambda e: e.tensor_tensor(out=spos[:], in0=PS[7][:, 0:32], in1=base[:], op=ALU.add), reads=[RP[7], R_rr], writes=[R_rr])
                k.op("dve", lambda e: e.tensor_tensor(out=base[:], in0=PS[7][:, 32:64], in1=base[:], op=ALU.add), reads=[RP[7], R_rr], writes=[R_rr])
                dv(lambda e: e.tensor_scalar(out=t32[:], in0=spos[:], scalar1=float(CAP), scalar2=1e6, op0=ALU.is_ge, op1=ALU.mult))
                dv(lambda e: e.tensor_tensor(out=spos[:], in0=spos[:], in1=t32[:], op=ALU.add))
                dv(lambda e: e.scalar_tensor_tensor(out=spos[:], in0=iota32, scalar=float(CAP), in1=spos[:], op0=ALU.mult, op1=ALU.add))
                for kk2, ohk in enumerate((oh1, oh2)):
                    dv(lambda e, ohk=ohk: e.tensor_tensor(out=t32[:], in0=ohk[:], in1=spos[:], op=ALU.mult))
                    dv(lambda e, kk2=kk2: e.reduce_sum(out=slf[:, kk2:kk2 + 1], in_=t32[:], axis=mybir.AxisListType.X))
                k.op("dve", lambda e: e.tensor_copy(out=sli[b][:], in_=slf[:]), reads=[R_rr], writes=[R_sli[b], R_rr])
                k.op("dve", lambda e: e.tensor_copy(out=slots_all[:, 2 * i:2 * i + 2], in_=sli[b][:]), reads=[R_sli[b]], writes=[R_sw])
                yield
                for kk2 in range(2):
                    k.dma("pool", None, None, reads=[R_h2b[b], R_sli[b]], writes=[R_xg], add=not (i == 0 and kk2 == 0),
                          fn=lambda e, kk2=kk2: e.indirect_dma_start(out=xg_d[:, :], out_offset=bass.IndirectOffsetOnAxis(ap=sli[b][:, kk2:kk2 + 1], axis=0),
                                                                     in_=hn2b[b][:, :], in_offset=None, bounds_check=bc_reg, oob_is_err=False))

            def adv(g, n=1):
                for _ in range(n):
                    try:
                        next(g)
                    except StopIteration:
                        return

            adv(stageA(0), 99)
            adv(stageA(1), 99)
            for i in range(NCH):
                ga = stageA(i + 2) if i + 2 < NCH else iter(())
                gb = stageB(i)
                adv(ga)
                adv(gb)
                adv(ga)
                adv(gb)
                adv(gb)
                adv(ga)
                adv(gb)
                adv(ga)
                adv(gb)
                adv(ga, 99)
                adv(gb, 99)
            k.barrier()
        if dbg:
            o = ddbg("slots", [128, NCH * 2], I32)
            k.dma("sp", o, slots_all[:], reads=[R_sw], writes=[Res()])
            o = ddbg("wts", [128, NCH * 2], F32)
            k.dma("sp", o, wts_all[:].rearrange("p a b -> p (a b)"), reads=[R_sw], writes=[Res()])
        if stop_after == "tail2":
            k.barrier()
            return nc, dbg_d

        convert_some(999)
        R_y = Res("y_d")
        with contextlib.ExitStack() as es:
            w1 = [k.sb(es, f"e_w1{i}", [128, KD, 512], BF16) for i in range(2)]
            w3 = [k.sb(es, f"e_w3{i}", [128, KD, 512], BF16) for i in range(2)]
            w2 = [k.sb(es, f"e_w2{i}", [128, 4, D], BF16) for i in range(2)]
            R_w = [Res(), Res()]
            X = [[k.sb(es, f"e_X{i}{j}", [128, D], BF16) for j in range(2)] for i in range(2)]
            R_X = [[Res(), Res()], [Res(), Res()]]
            XT = k.sb(es, "e_XT", [128, KD, 256], BF16); R_XT = Res()
            s1 = [k.sb(es, f"e_s1{i}", [128, 512], F32) for i in range(2)]; R_s1 = [Res(), Res()]
            actm = [k.sb(es, f"e_actm{i}", [128, 512], BF16) for i in range(2)]; R_am = [Res(), Res()]
            actT = k.sb(es, "e_actT", [128, 4, 256], BF16); R_at = [Res(), Res()]
            ys = [k.sb(es, f"e_ys{i}", [128, D], F32) for i in range(2)]; R_ys = [Res(), Res()]

            def load_e(e_):
                b = e_ % 2
                k.start_fill(R_w[b])
                for (dst_, src_, kc) in ((w1[b], w1b_d[e_], KD), (w3[b], w3b_d[e_], KD), (w2[b], w2b_d[e_], 4)):
                    v_ = src_.rearrange("(k p) c -> p k c", p=128)
                    hk = kc // 2
                    for k0 in (0, hk):
                        k.dma("sp", dst_[:, k0:k0 + hk, :], v_[:, k0:k0 + hk, :], reads=[R_wb], writes=[R_w[b]], add=True)
                for blk in range(2):
                    r0 = e_ * CAP + blk * 128
                    k.dma("sp", X[b][blk][:], xg_d[r0:r0 + 128, :], reads=[R_xg], writes=[R_X[b][blk]])
            load_e(0)
            yi = 0
            for e_ in range(32):
                if e_ + 1 < 32:
                    load_e(e_ + 1)
                b = e_ % 2
                for blk in range(2):
                    for hb in range(2):
                        pb = PSB[4 + hb]
                        k.group("pe", [TR(pb[:, j * 128:(j + 1) * 128], X[b][blk][:, (hb * 8 + j) * 128:(hb * 8 + j + 1) * 128], identb[:]) for j in range(8)],
                                reads=[R_X[b][blk], R_c], writes=[RP[4 + hb]])
                        src3 = pb.rearrange("p (a t) -> p a t", t=128)
                        dst3 = XT[:, hb * 8:(hb + 1) * 8, blk * 128:(blk + 1) * 128]
                        if hb == 0:
                            k.op("act", lambda e: e.copy(out=dst3, in_=src3), reads=[RP[4 + hb]], writes=[R_XT])
                        else:
                            k.op("dve", lambda e: e.tensor_copy(out=dst3, in_=src3), reads=[RP[4 + hb]], writes=[R_XT])
                for blk in range(2):
                    pa, pc_ = (2, 3) if blk == 0 else (6, 7)
                    bs = slice(blk * 128, (blk + 1) * 128)
                    k.group("pe", [MM(PS[pa][:], XT[:, kk, bs], w1[b][:, kk, :], kk == 0, kk == KD - 1) for kk in range(KD)],
                            reads=[R_w[b], R_XT], writes=[RP[pa]])
                    k.group("pe", [MM(PS[pc_][:], XT[:, kk, bs], w3[b][:, kk, :], kk == 0, kk == KD - 1) for kk in range(KD)],
                            reads=[R_w[b], R_XT], writes=[RP[pc_]])
                    k.op("act", lambda e: e.activation(out=s1[blk][:], in_=PS[pa][:], func=AF.Silu), reads=[RP[pa]], writes=[R_s1[blk]])
                    k.op("dve", lambda e: e.tensor_tensor(out=actm[blk][:], in0=PS[pc_][:], in1=s1[blk][:], op=ALU.mult),
                         reads=[RP[pc_], R_s1[blk]], writes=[R_am[blk]])
                    k.group("pe", [TR(PSB[4 + blk][:, fb * 128:(fb + 1) * 128], actm[blk][:, fb * 128:(fb + 1) * 128], identb[:]) for fb in range(4)],
                            reads=[R_am[blk], R_c], writes=[RP[4 + blk]])
                    src3 = PSB[4 + blk][:, 0:512].rearrange("p (a t) -> p a t", t=128)
                    if blk == 0:
                        k.op("act", lambda e: e.copy(out=actT[:, :, bs], in_=src3), reads=[RP[4 + blk]], writes=[R_at[blk]])
                    else:
                        k.op("dve", lambda e: e.tensor_copy(out=actT[:, :, bs], in_=src3), reads=[RP[4 + blk]], writes=[R_at[blk]])
                for blk in range(2):
                    yb = yi % 2
                    for nb in range(4):
                        pn = nb % 2
                        k.group("pe", [MM(PS[pn][:], actT[:, fb, blk * 128:(blk + 1) * 128], w2[b][:, fb, nb * 512:(nb + 1) * 512], fb == 0, fb == 3) for fb in range(4)],
                                reads=[R_at[blk], R_w[b]], writes=[RP[pn]])
                        if pn == 0:
                            k.op("act", lambda e: e.copy(out=ys[yb][:, nb * 512:(nb + 1) * 512], in_=PS[pn][:]), reads=[RP[pn]], writes=[R_ys[yb]], add=(nb > 0))
                        else:
                            k.op("dve", lambda e: e.tensor_copy(out=ys[yb][:, nb * 512:(nb + 1) * 512], in_=PS[pn][:]), reads=[RP[pn]], writes=[R_ys[yb]], add=True)
                    r0 = e_ * CAP + blk * 128
                    k.dma("sp", y_d[r0:r0 + 128, :], ys[yb][:], reads=[R_ys[yb]], writes=[R_y], add=True)
                    yi += 1
            k.barrier()
        if stop_after == "experts":
            k.barrier()
            return nc, dbg_d

        R_out = Res("out")
        with contextlib.ExitStack() as es:
            wpg = k.sb(es, "f_wpg", [128, KD, D], BF16); wpu = k.sb(es, "f_wpu", [128, 2, D], BF16); R_wp = Res()
            k.start_fill(R_wp)
            wv_ = wpgb_d.rearrange("(k p) c -> p k c", p=128)
            for k0 in range(0, KD, 2):
                k.dma("act" if (k0 // 2) % 2 else "sp", wpg[:, k0:k0 + 2, :], wv_[:, k0:k0 + 2, :], reads=[R_wbd], writes=[R_wp], add=True)
            k.dma("sp", wpu[:], wpub_d.rearrange("(k p) c -> p k c", p=128), reads=[R_wbd], writes=[R_wp], add=True)
            gprow = row_bcast(es, "gplerow", gple_d, D, R_c)
            gfrow = row_bcast(es, "gfinrow", gfin_d, D, R_c)
            x1t = [k.sb(es, f"f_x1{i}", [128, D], F32) for i in range(2)]; R_x1t = [Res(), Res()]
            x2t = [k.sb(es, f"f_x2{i}", [128, D], F32) for i in range(3)]; R_x2t = [Res(), Res(), Res()]
            ya = [k.sb(es, f"f_ya{i}", [128, D], F32) for i in range(2)]; R_ya = [Res(), Res()]
            yb_ = [k.sb(es, f"f_yb{i}", [128, D], F32) for i in range(2)]; R_yb = [Res(), Res()]
            pt = [k.sb(es, f"f_pt{i}", [128, 256], F32) for i in range(2)]; R_pt = [Res(), Res()]
            ptb = [k.sb(es, f"f_ptb{i}", [128, 256], BF16) for i in range(2)]; R_ptb = [Res(), Res()]
            pT = [k.sb(es, f"f_pT{i}", [128, 2, 128], BF16) for i in range(2)]; R_pT = [Res(), Res()]
            hb3 = [k.sb(es, f"f_hb3{i}", [128, D], BF16) for i in range(2)]; R_hb3 = [Res(), Res()]
            h3T = [k.sb(es, f"f_h3T{i}", [128, KD, 128], BF16) for i in range(2)]; R_h3T = [Res(), Res()]
            sg = [k.sb(es, f"f_sg{i}", [128, 512], F32) for i in range(2)]; R_sg = [Res(), Res()]
            tq = [k.sb(es, f"f_tq{i}", [128, 512], F32) for i in range(2)]; R_tq = [Res(), Res()]
            x3 = k.sb(es, "f_x3", [128, D], F32); R_x3 = Res()
            ot_ = k.sb(es, "f_ot", [128, D], F32); ot = [ot_, ot_]; R_ot_ = Res(); R_ot = [R_ot_, R_ot_]
            ssA = k.sb(es, "f_ssA", [128, 2], F32); R_sA = Res(); R_rA = Res()
            ssB = k.sb(es, "f_ssB", [128, 2], F32); R_sB = Res(); R_rB = Res()

            def stageG(i):
                b = i % 2
                rows = slice(i * 128, (i + 1) * 128)
                k.dma("sp", x1t[b][:], x1_d[rows, :], reads=[R_x1], writes=[R_x1t[b]])
                k.dma("sp", pt[b][:], pm[rows, :], writes=[R_pt[b]])
                k.op("pool", lambda e: e.memset(ya[b][:], 0.0), writes=[R_ya[b]])
                k.op("pool", lambda e: e.memset(yb_[b][:], 0.0), writes=[R_yb[b]])
                for kk2, (yt, Ry) in enumerate(((ya[b], R_ya[b]), (yb_[b], R_yb[b]))):
                    k.dma("pool", None, None, reads=[R_y, R_sw, Ry], writes=[Ry], add=True,
                          fn=lambda e, kk2=kk2, yt=yt: e.indirect_dma_start(out=yt[:, :], out_offset=None, in_=y_d[:, :],
                                                                           in_offset=bass.IndirectOffsetOnAxis(ap=slots_all[:, 2 * i + kk2:2 * i + kk2 + 1], axis=0),
                                                                           bounds_check=bc_reg, oob_is_err=False))

            def A1(i):
                b = i % 2
                t3 = i % 3
                k.op("dve", lambda e: e.scalar_tensor_tensor(out=x2t[t3][:], in0=ya[b][:], scalar=wts_all[:, i, 0:1], in1=x1t[b][:], op0=ALU.mult, op1=ALU.add),
                     reads=[R_ya[b], R_sw, R_x1t[b]], writes=[R_x2t[t3]])
                k.op("dve", lambda e: e.scalar_tensor_tensor(out=x2t[t3][:], in0=yb_[b][:], scalar=wts_all[:, i, 1:2], in1=x2t[t3][:], op0=ALU.mult, op1=ALU.add),
                     reads=[R_yb[b], R_sw, R_x2t[t3]], writes=[R_x2t[t3]])
                if dbg and i == 0:
                    o = ddbg("x2t0", [128, D])
                    k.dma("sp", o, x2t[t3][:], reads=[R_x2t[t3]], writes=[Res()])

            def A2(i):
                b = i % 2
                t3 = i % 3
                k.op("act", lambda e: e.activation(out=hb3[b][:], in_=x2t[t3][:], func=AF.Square, accum_out=ssA[:, 0:1]), reads=[R_x2t[t3]], writes=[R_hb3[b], R_sA])
                rstd_from_ss(ssA[:, 0:1], ssA[:, 1:2], R_sA, R_rA, D)
                k.op("dve", lambda e: e.scalar_tensor_tensor(out=hb3[b][:], in0=x2t[t3][:], scalar=ssA[:, 1:2], in1=gprow[:], op0=ALU.mult, op1=ALU.mult),
                     reads=[R_x2t[t3], R_rA, R_c], writes=[R_hb3[b]])
                k.op("dve", lambda e: e.tensor_copy(out=ptb[b][:], in_=pt[b][:]), reads=[R_pt[b]], writes=[R_ptb[b]])

            def A3(i):
                b = i % 2
                t3 = i % 3
                for hb in range(2):
                    pb = PSB[4 + hb]
                    k.group("pe", [TR(pb[:, j * 128:(j + 1) * 128], hb3[b][:, (hb * 8 + j) * 128:(hb * 8 + j + 1) * 128], identb[:]) for j in range(8)],
                            reads=[R_hb3[b], R_c], writes=[RP[4 + hb]])
                    k.op("act", lambda e: e.copy(out=h3T[b][:, hb * 8:(hb + 1) * 8, :], in_=pb.rearrange("p (a t) -> p a t", t=128)),
                         reads=[RP[4 + hb]], writes=[R_h3T[b]])
                k.group("pe", [TR(PSB[6][:, j * 128:(j + 1) * 128], ptb[b][:, j * 128:(j + 1) * 128], identb[:]) for j in range(2)],
                        reads=[R_ptb[b], R_c], writes=[RP[6]])
                k.op("dve", lambda e: e.tensor_copy(out=pT[b][:], in_=PSB[6][:, 0:256].rearrange("p (a t) -> p a t", t=128)), reads=[RP[6]], writes=[R_pT[b]])

            def Bnb(i, nb):
                b = i % 2
                t3 = i % 3
                ns = slice(nb * 512, (nb + 1) * 512)
                pg = nb % 2
                gb_ = (0, 1, 7)[(i * 4 + nb) % 3]
                k.group("pe", [MM(PS[gb_][:], h3T[b][:, kk, :], wpg[:, kk, ns], kk == 0, kk == KD - 1) for kk in range(KD)],
                        reads=[R_h3T[b], R_wp], writes=[RP[gb_]])
                k.group("pe", [MM(PS[2 + pg][:], pT[b][:, kk, :], wpu[:, kk, ns], kk == 0, kk == 1) for kk in range(2)],
                        reads=[R_pT[b], R_wp], writes=[RP[2 + pg]])
                k.op("act", lambda e: e.activation(out=sg[pg][:], in_=PS[gb_][:], func=AF.Sigmoid), reads=[RP[gb_]], writes=[R_sg[pg]])
                k.op("dve", lambda e: e.tensor_tensor(out=tq[pg][:], in0=PS[2 + pg][:], in1=sg[pg][:], op=ALU.mult), reads=[RP[2 + pg], R_sg[pg]], writes=[R_tq[pg]])
                k.op("pool", lambda e: e.tensor_tensor(out=x3[:, ns], in0=tq[pg][:], in1=x2t[t3][:, ns], op=ALU.add), reads=[R_tq[pg], R_x2t[t3]], writes=[R_x3])

            def Bfin(i):
                b = i % 2
                rows = slice(i * 128, (i + 1) * 128)
                k.op("act", lambda e: e.activation(out=ot[b][:], in_=x3[:], func=AF.Square, accum_out=ssB[:, 0:1]), reads=[R_x3], writes=[R_ot[b], R_sB])
                rstd_from_ss(ssB[:, 0:1], ssB[:, 1:2], R_sB, R_rB, D)
                k.op("dve", lambda e: e.scalar_tensor_tensor(out=ot[b][:], in0=x3[:], scalar=ssB[:, 1:2], in1=gfrow[:], op0=ALU.mult, op1=ALU.mult),
                     reads=[R_x3, R_rB, R_c], writes=[R_ot[b]])
                k.dma("sp", out_d[rows, :], ot[b][:], reads=[R_ot[b]], writes=[R_out], add=True)

            stageG(0); stageG(1)
            A1(0); A2(0); A3(0)
            stageG(2)
            A1(1); A2(1)
            for i in range(NCH):
                Bnb(i, 0)
                if i + 2 < NCH:
                    A1(i + 2)
                Bnb(i, 1)
                if i + 2 < NCH:
                    A2(i + 2)
                Bnb(i, 2)
                if i + 3 < NCH:
                    stageG(i + 3)
                if i + 1 < NCH:
                    A3(i + 1)
                Bnb(i, 3)
                Bfin(i)
            k.barrier()
    return nc, dbg_d


def host_consts():
    c = np.zeros((128, 6, 128), np.float32)
    s = np.arange(128)[:, None]; t = np.arange(128)[None, :]
    c[:, 0, :] = np.eye(128)
    c[:, 1, :] = (s <= t)
    c[:, 2, :] = 1.0
    c[:, 3, :] = np.where(s <= t, 0.0, -30000.0)
    c[:, 4, :] = (s < t)
    c[:, 5, :] = np.arange(128)[None, :]
    return c


def make_in_maps(inp, cores=range(8)):
    x = np.asarray(inp["x"], np.float32); p = np.asarray(inp["p"], np.float32)[0]
    g = lambda n: np.ascontiguousarray(np.asarray(inp[n], np.float32)[0])
    conv_w = g("conv_w"); conv_b = g("conv_b")
    shared = {
        "w_in": g("w_in"),
        "convw": np.ascontiguousarray(conv_w.reshape(4, 16, 128).transpose(2, 1, 0)),
        "convb": np.ascontiguousarray(conv_b.reshape(16, 128).T),
        "b_gate": g("b_gate"), "gn_m": g("gn_m"), "ln_g": g("ln_g"), "ln_b": g("ln_b"),
        "w_s": g("w_s"), "b_s": np.ascontiguousarray(g("b_s").reshape(1024)),
        "w_bm": g("w_bm"), "w_bg": g("w_bg"), "w_out": g("w_out"),
        "g_mix": g("g_mix"), "g_ffn": g("g_ffn"), "g_ple": g("g_ple"),
        "g_final": np.ascontiguousarray(np.asarray(inp["g_final"], np.float32)),
        "w_r": np.ascontiguousarray(np.concatenate([g("w_rg"), g("w_re")], axis=1)),
        "b_r": np.ascontiguousarray(np.concatenate([g("b_rg"), g("b_re")], axis=0)),
        "w1": g("w1"), "w3": g("w3"), "w2": g("w2"),
        "w_ple_up": g("w_ple_up"), "w_ple_gate": g("w_ple_gate"),
        "consts": host_consts(),
    }
    maps = []
    for c in cores:
        b, half = c // 2, c % 2
        m = dict(shared)
        m["xm"] = np.ascontiguousarray(x[b, half * NT:(half + 1) * NT])
        m["xp"] = np.ascontiguousarray(x[b, 0:NT])
        m["xh"] = np.ascontiguousarray(x[b, NT - 128:NT]) if half == 1 else np.zeros((128, D), np.float32)
        m["flag"] = np.full((128, 1), float(half), np.float32)
        m["pm"] = np.ascontiguousarray(p[b, half * NT:(half + 1) * NT])
        maps.append(m)
    return maps


def kernel(**inputs):
    nc, _ = build()
    maps = make_in_maps(inputs)
    res = run_bass_kernel_spmd(nc, maps, core_ids=list(range(8)))
    out = np.zeros((4, 4096, D), np.float32)
    for c in range(8):
        b, half = c // 2, c % 2
        out[b, half * NT:(half + 1) * NT] = np.asarray(res.results[c]["out"], np.float32)
    return out
```

```python
import contextlib
import numpy as np
import ml_dtypes
import concourse.bass as bass
import concourse.mybir as mybir
from concourse.bass_utils import run_bass_kernel_spmd

F32 = mybir.dt.float32
BF16 = mybir.dt.bfloat16
I32 = mybir.dt.int32
AF = mybir.ActivationFunctionType
ALU = mybir.AluOpType

D = 2048
NT = 2048
NCH = 16
KD = 16
EPS = 1e-6
CAP = 256
NSLOT = 32 * CAP
IN_COLS = 10248
C_Q, C_K, C_V, C_O, C_IF, C_U, C_VG, C_GM, C_GG = 0, 1024, 2048, 3072, 4096, 4104, 5128, 6152, 8200
HT = 3 + NT
VW = 264


class Res:
    __slots__ = ("name", "w", "r", "pr")

    def __init__(self, name=""):
        self.name = name
        self.w = {}
        self.r = {}
        self.pr = {}


class KB:
    def __init__(self, nc, es, ndma=(8, 8, 4)):
        self.nc = nc
        self.es = es
        self.eng = {"pe": nc.tensor, "act": nc.scalar, "dve": nc.vector,
                    "pool": nc.gpsimd, "sp": nc.sync}
        self.sem = {}
        self.cnt = {}
        self.last = {}
        self.seen = {e: {} for e in self.eng}
        for e in self.eng:
            self.sem[e] = es.enter_context(nc.semaphore("sem_" + e))
            self.cnt[e] = 0
        self.eng["cv"] = nc.gpsimd
        self.seen["cv"] = {}
        self.dq = {}
        ndma = tuple(ndma) + (8,)
        for q, n in zip(("sp", "pool", "act", "cv"), ndma):
            sems = [es.enter_context(nc.semaphore(f"dsem_{q}{i}")) for i in range(n)]
            self.dq[q] = {"sems": sems, "i": 0, "cnt": [0] * n}
            for i, s in enumerate(sems):
                self.sem[(q, i)] = s
        self.ninst = 0

    def _wait(self, e, ev):
        if ev is None:
            return
        key, val = ev
        if self.seen[e].get(key, 0) >= val:
            return
        self.eng[e].wait_ge(self.sem[key], val)
        self.seen[e][key] = val
        self.ninst += 1

    def start_fill(self, res):
        pr = dict(res.r)
        for kk, v in res.w.items():
            if pr.get(kk, 0) < v:
                pr[kk] = v
        res.pr = pr
        res.r = {}
        res.w = {}

    def _deps(self, e, reads, writes, add=False):
        for r in reads:
            for kv in list(r.w.items()):
                self._wait(e, kv)
        for w in writes:
            if not add:
                self.start_fill(w)
            for kv in list(w.pr.items()):
                self._wait(e, kv)

    def _commit(self, ev, reads, writes):
        for r in reads:
            if r.r.get(ev[0], 0) < ev[1]:
                r.r[ev[0]] = ev[1]
        for w in writes:
            if w.w.get(ev[0], 0) < ev[1]:
                w.w[ev[0]] = ev[1]

    def op(self, e, fn, reads=(), writes=(), add=False):
        self._deps(e, reads, writes, add)
        ins = fn(self.eng[e])
        self.cnt[e] += 1
        ins.then_inc(self.sem[e], 1)
        ev = (e, self.cnt[e])
        self._commit(ev, reads, writes)
        self.ninst += 1
        return ev

    def group(self, e, fns, reads=(), writes=(), add=False):
        self._deps(e, reads, writes, add)
        ins = None
        for fn in fns:
            ins = fn(self.eng[e])
            self.ninst += 1
        self.cnt[e] += 1
        ins.then_inc(self.sem[e], 1)
        ev = (e, self.cnt[e])
        self._commit(ev, reads, writes)
        return ev

    def dma(self, q, out, in_, reads=(), writes=(), fn=None, add=False):
        self._deps(q, reads, writes, add)
        d = self.dq[q]
        i = d["i"]
        d["i"] = (i + 1) % len(d["sems"])
        key = (q, i)
        if d["cnt"][i] > 0:
            self._wait(q, (key, d["cnt"][i]))
        if fn is None:
            ins = self.eng[q].dma_start(out=out, in_=in_)
        else:
            ins = fn(self.eng[q])
        d["cnt"][i] += 16
        ins.then_inc(d["sems"][i], 16)
        ev = (key, d["cnt"][i])
        self._commit(ev, reads, writes)
        self.ninst += 1
        return ev

    def barrier(self):
        evs = [(e, self.cnt[e]) for e in self.cnt if self.cnt[e] > 0]
        for q, d in self.dq.items():
            if q == "cv":
                continue
            for i, c in enumerate(d["cnt"]):
                if c > 0:
                    evs.append(((q, i), c))
        for e in self.eng:
            for ev in evs:
                self._wait(e, ev)

    def sb(self, es, name, shape, dt):
        self.uid = getattr(self, "uid", 0) + 1
        return es.enter_context(self.nc.sbuf_tensor(f"sb{self.uid}_{name}", list(shape), dt))

    def ps(self, es, name, shape, dt):
        self.uid = getattr(self, "uid", 0) + 1
        return es.enter_context(self.nc.psum_tensor(f"pp{self.uid}_{name}", list(shape), dt))


def MM(out, lhsT, rhs, start, stop):
    return lambda e: e.matmul(out, lhsT=lhsT, rhs=rhs, start=start, stop=stop)


def TR(out, in_, ident):
    return lambda e: e.transpose(out=out, in_=in_, identity=ident)


def build(dbg=False, stop_after=None, CUT=None):
    nc = bass.Bass("TRN2", target_bir_lowering=False)

    def din(name, shape, dt=F32):
        return nc.dram_tensor(name, list(shape), dt, kind="ExternalInput").ap()

    def dscr(name, shape, dt):
        kind = "ExternalOutput" if dbg else "Internal"
        return nc.dram_tensor(name, list(shape), dt, kind=kind).ap()

    xm = din("xm", [NT, D]); xp = din("xp", [NT, D]); xh = din("xh", [128, D])
    flag_d = din("flag", [128, 1]); pm = din("pm", [NT, 256])
    w_in = din("w_in", [D, IN_COLS])
    convw_d = din("convw", [128, 16, 4]); convb_d = din("convb", [128, 16])
    bgate_d = din("b_gate", [8]); gnm_d = din("gn_m", [1024])
    lng_d = din("ln_g", [1024]); lnb_d = din("ln_b", [1024])
    ws_d = din("w_s", [8, 128, 128]); bs_d = din("b_s", [1024])
    wbm_d = din("w_bm", [1024, D]); wbg_d = din("w_bg", [1024, D]); wout_d = din("w_out", [D, D])
    gmix_d = din("g_mix", [D]); gffn_d = din("g_ffn", [D]); gple_d = din("g_ple", [D]); gfin_d = din("g_final", [D])
    wr_d = din("w_r", [D, 36]); br_d = din("b_r", [36])
    w1_d = din("w1", [32, D, 512]); w3_d = din("w3", [32, D, 512]); w2_d = din("w2", [32, 512, D])
    wpu_d = din("w_ple_up", [256, D]); wpg_d = din("w_ple_gate", [D, D])
    cst_d = din("consts", [128, 6, 128])
    out_d = nc.dram_tensor("out", [NT, D], F32, kind="ExternalOutput").ap()

    x1_d = dscr("x1_s", [NT, D], F32)
    hgT_d = dscr("hgT_s", [8, 128, NT], BF16)
    hmT_d = dscr("hmT_s", [8, 128, NT], BF16)
    mgT_d = dscr("mgT_s", [16, 128, NT], BF16)
    xg_d = dscr("xg_s", [NSLOT, D], BF16)
    y_d = dscr("y_s", [NSLOT, D], F32)
    w1b_d = nc.dram_tensor("w1b_s", [32, D, 512], BF16, kind="Internal").ap()
    w3b_d = nc.dram_tensor("w3b_s", [32, D, 512], BF16, kind="Internal").ap()
    w2b_d = nc.dram_tensor("w2b_s", [32, 512, D], BF16, kind="Internal").ap()
    dbg_d = {}

    def ddbg(name, shape, dt=F32):
        if dbg:
            dbg_d[name] = nc.dram_tensor("dbg_" + name, list(shape), dt, kind="ExternalOutput").ap()
            return dbg_d[name]
        return None

    with contextlib.ExitStack() as es0:
        k = KB(nc, es0)
        cstf = k.sb(es0, "cstf", [128, 6, 128], F32)
        identb = k.sb(es0, "identb", [128, 128], BF16)
        flag = k.sb(es0, "flag", [128, 1], F32)
        slots_all = k.sb(es0, "slots_all", [128, NCH * 2], I32)
        wts_all = k.sb(es0, "wts_all", [128, NCH, 2], F32)
        R_c = Res("consts"); R_C = Res("C"); R_Cb = Res("Cb"); R_sw = Res("slotw")
        k.dma("sp", cstf[:], cst_d[:], writes=[R_c], add=True)
        k.dma("sp", flag[:], flag_d[:], writes=[R_c], add=True)
        k.op("dve", lambda e: e.tensor_copy(out=identb[:], in_=cstf[:, 0, :]), reads=[R_c], writes=[R_c], add=True)
        mhalf = k.sb(es0, "mhalf", [128, 16], F32)
        k.op("dve", lambda e: e.memset(mhalf[:], -0.5), writes=[R_c], add=True)
        identf = cstf[:, 0, :]; U = cstf[:, 1, :]; ones = cstf[:, 2, :]; negmT = cstf[:, 3, :]
        Ustr = cstf[:, 4, :]; iota32 = cstf[:, 5, 0:32]

        PS = [k.ps(es0, f"ps{i}", [128, 512], F32) for i in range(8)]
        PSB = [PS[i][:].bitcast(BF16) for i in range(8)]
        RP = [Res(f"ps{i}") for i in range(8)]

        R_z = Res("z"); R_xg = Res("xg")
        bc_reg = nc.gpsimd.to_reg(NSLOT - 1)

        def rstd_from_ss(ss, rs, R_ss, R_rs, n):
            k.op("dve", lambda e: e.tensor_scalar(out=rs, in0=ss, scalar1=1.0 / n, scalar2=EPS,
                                                  op0=ALU.mult, op1=ALU.add), reads=[R_ss], writes=[R_rs])
            k.op("pool", lambda e: e.tensor_tensor(out=rs, in0=rs, in1=mhalf[:, 0:1], op=ALU.pow), reads=[R_rs, R_c], writes=[R_rs])

        def norm_phase(tiles, grow, R_grow, hnT, R_hn):
            with contextlib.ExitStack() as es:
                xb = [k.sb(es, f"n_x{i}", [128, D], F32) for i in range(2)]
                junk = k.sb(es, "n_junk", [128, D], BF16)
                xs = [k.sb(es, f"n_xs{i}", [128, D], BF16) for i in range(2)]
                ss = k.sb(es, "n_ss", [128, 2], F32)
                R_x = [Res(), Res()]; R_j = Res(); R_xs = [Res(), Res()]; R_s = Res(); R_r = Res()
                for ti, (src, col0, nuse, rkey) in enumerate(tiles):
                    b = ti % 2
                    k.dma("sp", xb[b][:], src, writes=[R_x[b]])
                    k.op("act", lambda e: e.activation(out=junk[:], in_=xb[b][:], func=AF.Square,
                                                       accum_out=ss[:, 0:1]), reads=[R_x[b]], writes=[R_j, R_s])
                    rstd_from_ss(ss[:, 0:1], ss[:, 1:2], R_s, R_r, D)
                    k.op("dve", lambda e: e.scalar_tensor_tensor(out=xs[b][:], in0=xb[b][:], scalar=ss[:, 1:2],
                                                                 in1=grow[:], op0=ALU.mult, op1=ALU.mult),
                         reads=[R_x[b], R_r, R_grow], writes=[R_xs[b]])
                    for hb in range(2):
                        pb = PSB[hb + 2 * b]
                        k.group("pe", [TR(pb[:, j * 128:(j + 1) * 128], xs[b][:, (hb * 8 + j) * 128:(hb * 8 + j + 1) * 128],
                                          identb[:]) for j in range(8)], reads=[R_xs[b], R_c], writes=[RP[hb + 2 * b]])
                        src3 = pb.rearrange("p (a t) -> p a t", t=128)[:, :, 128 - nuse:128]
                        dst3 = hnT[:, hb * 8:(hb + 1) * 8, col0:col0 + nuse]
                        eng = "act" if hb == 0 else "dve"
                        if eng == "act":
                            k.op("act", lambda e: e.copy(out=dst3, in_=src3), reads=[RP[hb + 2 * b]], writes=[R_hn[rkey]])
                        else:
                            k.op("dve", lambda e: e.tensor_copy(out=dst3, in_=src3), reads=[RP[hb + 2 * b]], writes=[R_hn[rkey]])
                k.barrier()

        def load_w_cast(dst, src_rows_cols, kchunks, R_dst, step=4, new_fill=True):
            v = src_rows_cols.rearrange("(k p) c -> p k c", p=128)
            if new_fill:
                k.start_fill(R_dst)
            for k0 in range(0, kchunks, step):
                k1 = min(kchunks, k0 + step)
                k.dma("pool", dst[:, k0:k1, :], v[:, k0:k1, :], writes=[R_dst], add=True)

        R_wb = Res("wb")
        conv_list = [(w1_d, w1b_d, e_) for e_ in range(32)] + [(w3_d, w3b_d, e_) for e_ in range(32)]
        conv_list = [x for pair in zip(conv_list[:32], conv_list[32:]) for x in pair] + [(w2_d, w2b_d, e_) for e_ in range(32)]
        conv_pos = [0]

        def convert_some(n):
            for _ in range(n):
                if conv_pos[0] >= len(conv_list):
                    return
                src, dst, e_ = conv_list[conv_pos[0]]
                conv_pos[0] += 1
                k.dma("cv", dst[e_].rearrange("(k p) c -> p k c", p=128), src[e_].rearrange("(k p) c -> p k c", p=128), writes=[R_wb], add=True)

        def row_bcast(es, name, src1d, n, R):
            t = k.sb(es, name, [128, n], F32)
            k.dma("sp", t[:], src1d.partition_broadcast(128), writes=[R], add=True)
            return t

        def gate_prep(es, hnT, R_hn, wif, R_wif, bgrow, R_bg):
            G = {}
            gsb = k.sb(es, "g_gsb", [128, NCH, 8], F32)
            lf = k.sb(es, "g_lf", [128, NCH, 4], F32)
            bcol = k.sb(es, "g_bcol", [128, NCH, 4], F32)
            biasc = k.sb(es, "g_biasc", [128, NCH, 4], F32)
            gcol = k.sb(es, "g_gcol", [128, NCH, 4], F32)
            wcol = k.sb(es, "g_wcol", [128, NCH, 4], F32)
            egcol = k.sb(es, "g_egcol", [128, NCH, 4], F32)
            R_g = Res("gates")
            for c in range(NCH):
                k.group("pe", [MM(PS[4][:, 0:8], hnT[:, kk, 3 + c * 128:3 + (c + 1) * 128], wif[:, kk, :], kk == 0, kk == KD - 1)
                               for kk in range(KD)], reads=[R_hn[c], R_wif], writes=[RP[4]])
                k.op("dve", lambda e: e.tensor_tensor(out=gsb[:, c, :], in0=PS[4][:, 0:8], in1=bgrow[:], op=ALU.add),
                     reads=[RP[4], R_bg], writes=[R_g])
            k.op("act", lambda e: e.activation(out=lf[:], in_=gsb[:, :, 4:8], func=AF.Exp, scale=-1.0), reads=[R_g], writes=[R_g])
            k.op("act", lambda e: e.activation(out=lf[:], in_=lf[:], func=AF.Ln, bias=1.0), reads=[R_g], writes=[R_g])
            k.op("dve", lambda e: e.tensor_scalar_mul(out=lf[:], in0=lf[:], scalar1=-1.0), reads=[R_g], writes=[R_g])
            lf2 = lf[:].rearrange("p c h -> p (c h)")
            k.op("pe", MM(PS[4][:, 0:64], U, lf2, True, True), reads=[R_g, R_c], writes=[RP[4]])
            k.op("dve", lambda e: e.tensor_copy(out=bcol[:].rearrange("p c h -> p (c h)"), in_=PS[4][:, 0:64]), reads=[RP[4]], writes=[R_g])
            k.op("pe", MM(PS[4][:, 0:64], ones, lf2, True, True), reads=[R_g, R_c], writes=[RP[4]])
            k.op("dve", lambda e: e.tensor_copy(out=gcol[:].rearrange("p c h -> p (c h)"), in_=PS[4][:, 0:64]), reads=[RP[4]], writes=[R_g])
            k.op("dve", lambda e: e.tensor_tensor(out=biasc[:], in0=gsb[:, :, 0:4], in1=bcol[:], op=ALU.subtract), reads=[R_g], writes=[R_g])
            k.op("dve", lambda e: e.tensor_tensor(out=wcol[:], in0=biasc[:], in1=gcol[:], op=ALU.add), reads=[R_g], writes=[R_g])
            k.op("act", lambda e: e.activation(out=wcol[:], in_=wcol[:], func=AF.Exp), reads=[R_g], writes=[R_g])
            k.op("act", lambda e: e.activation(out=egcol[:], in_=gcol[:], func=AF.Exp), reads=[R_g], writes=[R_g])
            G.update(lf=lf, biasc=biasc, wcol=wcol, egcol=egcol, R=R_g)
            return G

        def conv_proj(es, tag, wq, R_wq, colblk0, hnT, R_hn, outT, R_out, cw, cb, R_cw, scale, halo, CP):
            if "pc" not in CP:
                CP["pc"] = k.sb(es, tag + "_pc", [128, 3 + 512], F32)
                CP["acc"] = k.sb(es, tag + "_acc", [128, 512], F32)
                CP["R"] = (Res(), Res())
            pc = CP["pc"]; acc = CP["acc"]; R_pc, R_acc = CP["R"]
            for blk in range(2):
                cblk = colblk0 + blk
                if halo:
                    k.group("pe", [MM(PS[6][:, 0:3], wq[:, kk, blk * 128:(blk + 1) * 128], hnT[:, kk, 0:3], kk == 0, kk == KD - 1)
                                   for kk in range(KD)], reads=[R_hn["halo"], R_wq], writes=[RP[6]])
                    k.op("act", lambda e: e.copy(out=pc[:, 0:3], in_=PS[6][:, 0:3]), reads=[RP[6]], writes=[R_pc])
                else:
                    k.op("dve", lambda e: e.memset(pc[:, 0:3], 0.0), writes=[R_pc])
                for tt in range(4):
                    pb = 6 + (tt % 2)
                    k.group("pe", [MM(PS[pb][:], wq[:, kk, blk * 128:(blk + 1) * 128], hnT[:, kk, 3 + tt * 512:3 + (tt + 1) * 512],
                                      kk == 0, kk == KD - 1) for kk in range(KD)],
                            reads=[R_hn[tt * 4 + j] for j in range(4)] + [R_wq], writes=[RP[pb]])
                    k.op("act", lambda e: e.copy(out=pc[:, 3:515], in_=PS[pb][:]), reads=[RP[pb]], writes=[R_pc])
                    k.op("dve", lambda e: e.tensor_scalar_mul(out=acc[:], in0=pc[:, 0:512], scalar1=cw[:, cblk, 0:1]),
                         reads=[R_pc, R_cw], writes=[R_acc])
                    for j in range(1, 4):
                        k.op("dve", lambda e: e.scalar_tensor_tensor(out=acc[:], in0=pc[:, j:j + 512], scalar=cw[:, cblk, j:j + 1],
                                                                     in1=acc[:], op0=ALU.mult, op1=ALU.add),
                             reads=[R_pc, R_cw, R_acc], writes=[R_acc])
                    dst = outT[:, blk, tt * 512:(tt + 1) * 512]
                    k.op("act", lambda e: e.activation(out=dst, in_=acc[:], func=AF.Silu, bias=cb[:, cblk:cblk + 1]),
                         reads=[R_acc, R_cw], writes=[R_out])
                    if scale != 1.0:
                        k.op("dve", lambda e: e.tensor_scalar_mul(out=dst, in0=dst, scalar1=scale), reads=[R_out], writes=[R_out])
                    k.op("dve", lambda e: e.tensor_copy(out=pc[:, 0:3], in_=pc[:, 512:515]), reads=[R_pc], writes=[R_pc])
                    yield

        def state_update(T, h, c, G, kT, R_kT, vaug, R_va):
            pbT = PSB[5]
            k.group("pe", [TR(pbT[:, blk * 128:(blk + 1) * 128], kT[:, blk, c * 128:(c + 1) * 128], identb[:]) for blk in range(2)],
                    reads=[R_kT, R_c], writes=[RP[5]])
            k.op("act", lambda e: e.copy(out=T["ktok"][:], in_=pbT[:, 0:256]), reads=[RP[5]], writes=[T["R_ktok"]])
            k.op("dve", lambda e: e.tensor_scalar_mul(out=T["wv"][:], in0=vaug[:], scalar1=G["wcol"][:, c, h:h + 1]),
                 reads=[R_va, G["R"]], writes=[T["R_wv"]])
            for blk in range(2):
                k.op("pe", MM(PS[2 + blk][:, 0:257], T["ktok"][:, blk * 128:(blk + 1) * 128], T["wv"][:], True, True),
                     reads=[T["R_ktok"], T["R_wv"]], writes=[RP[2 + blk]])
                k.op("dve", lambda e: e.scalar_tensor_tensor(out=Cst[:, h, blk, :], in0=Cst[:, h, blk, :],
                                                             scalar=G["egcol"][:, c, h:h + 1], in1=PS[2 + blk][:, 0:257],
                                                             op0=ALU.mult, op1=ALU.add),
                     reads=[RP[2 + blk], G["R"], R_C], writes=[R_C])

        def vproj(hnT, R_hn, c, wv, R_wv, vaug, R_va):
            k.group("pe", [MM(PS[0][:, 0:256], hnT[:, kk, 3 + c * 128:3 + (c + 1) * 128], wv[:, kk, :], kk == 0, kk == KD - 1)
                           for kk in range(KD)], reads=[R_hn[c], R_wv], writes=[RP[0]])
            k.op("act", lambda e: e.copy(out=vaug[:, 0:256], in_=PS[0][:, 0:256]), reads=[RP[0]], writes=[R_va])

        def chunk_temps(es):
            T = {}
            T["ktok"] = k.sb(es, "t_ktok", [128, 256], BF16); T["R_ktok"] = Res()
            T["wv"] = k.sb(es, "t_wv", [128, 257], BF16); T["R_wv"] = Res()
            T["vaug"] = [k.sb(es, f"t_vaug{i}", [128, 257], BF16) for i in range(2)]
            T["R_va"] = [Res(), Res()]
            for i in range(2):
                k.op("dve", lambda e: e.memset(T["vaug"][i][:, 256:257], 1.0), writes=[T["R_va"][i]])
            return T


        def cols(c):
            return slice(3 + c * 128, 3 + (c + 1) * 128)

        def batch_V(hnT, R_hn, wv, R_wv, wo, R_wo, vaug_all, R_vac, sig_all, R_sigc):
            for c in range(NCH):
                pb = c % 2
                k.group("pe", [MM(PS[pb][:, 0:256], hnT[:, kk, cols(c)], wv[:, kk, :], kk == 0, kk == KD - 1) for kk in range(KD)],
                        reads=[R_hn[c], R_wv], writes=[RP[pb]])
                if wo is not None:
                    k.group("pe", [MM(PS[pb][:, 256:512], hnT[:, kk, cols(c)], wo[:, kk, :], kk == 0, kk == KD - 1) for kk in range(KD)],
                            reads=[R_hn[c], R_wo], writes=[RP[pb]], add=True)
                k.op("act", lambda e: e.copy(out=vaug_all[:, c, 0:256], in_=PS[pb][:, 0:256]), reads=[RP[pb]], writes=[R_vac[c]])
                if wo is not None:
                    k.op("act", lambda e: e.activation(out=sig_all[:, c, :], in_=PS[pb][:, 256:512], func=AF.Sigmoid), reads=[RP[pb]], writes=[R_sigc[c]])

        def batch_K(h, G, kT, R_kT, ktok_all, R_ktc, vaug_all, R_vac, wv_all, R_wvc):
            for c4 in range(4):
                bank = 6 + c4 % 2
                k.group("pe", [TR(PSB[bank][:, (j * 2 + blk) * 128:(j * 2 + blk + 1) * 128], kT[:, blk, (c4 * 4 + j) * 128:(c4 * 4 + j + 1) * 128], identb[:])
                               for j in range(4) for blk in range(2)], reads=[R_kT, R_c], writes=[RP[bank]])
                dst = ktok_all[:, c4 * 4:(c4 + 1) * 4, :]
                src = PSB[bank][:, 0:1024].rearrange("p (a b) -> p a b", b=256)
                if c4 % 2 == 0:
                    k.op("act", lambda e: e.copy(out=dst, in_=src), reads=[RP[bank]], writes=[R_ktc[c4]])
                else:
                    k.op("dve", lambda e: e.tensor_copy(out=dst, in_=src), reads=[RP[bank]], writes=[R_ktc[c4]])
            for c in range(NCH):
                k.op("dve", lambda e: e.tensor_scalar_mul(out=wv_all[:, c, 0:257], in0=vaug_all[:, c, 0:257], scalar1=G["wcol"][:, c, h:h + 1]),
                     reads=[R_vac[c], G["R"]], writes=[R_wvc[c]])

        def kv_mm(c, ktok_all, R_ktc, wv_all, R_wvc):
            for blk in range(2):
                bank = (c % 2) * 2 + blk
                k.op("pe", MM(PS[bank][:, 0:257], ktok_all[:, c, blk * 128:(blk + 1) * 128], wv_all[:, c, 0:257], True, True),
                     reads=[R_ktc[c // 4], R_wvc[c]], writes=[RP[bank]])

        def c_update(h, c, G, Cst, R_Ch):
            for blk in range(2):
                bank = (c % 2) * 2 + blk
                k.op("dve", lambda e: e.scalar_tensor_tensor(out=Cst[:, h, blk, :], in0=Cst[:, h, blk, :], scalar=G["egcol"][:, c, h:h + 1],
                                                             in1=PS[bank][:, 0:257], op0=ALU.mult, op1=ALU.add),
                     reads=[RP[bank], G["R"], R_Ch], writes=[R_Ch])

        with contextlib.ExitStack() as es1:
            hnT = k.sb(es1, "hnT", [128, KD, HT], BF16)
            Cst = k.sb(es1, "Cst", [128, 4, 2, 257], F32)
            k.op("dve", lambda e: e.memset(Cst[:], 0.0), writes=[R_C])
            R_hn = {c: Res(f"hn{c}") for c in range(NCH)}
            R_hn["halo"] = Res("hnhalo")
            wif = k.sb(es1, "wif", [128, KD, 8], BF16); R_wif = Res()
            load_w_cast(wif, w_in[:, C_IF:C_IF + 8], KD, R_wif, step=16)
            bgrow = row_bcast(es1, "bgrow", bgate_d, 8, R_c)
            cw = k.sb(es1, "cw", [128, 16, 4], F32); cb = k.sb(es1, "cb", [128, 16], F32); R_cw = Res()
            k.dma("sp", cw[:], convw_d[:], writes=[R_cw], add=True); k.dma("sp", cb[:], convb_d[:], writes=[R_cw], add=True)

            with contextlib.ExitStack() as es:
                grow = row_bcast(es, "gmixrow", gmix_d, D, R_c)
                zt = k.sb(es, "zt", [128, D], BF16)
                k.op("pool", lambda e: e.memset(zt[:], 0.0), writes=[R_z])
                for i in range(NSLOT // 128):
                    k.dma("sp", xg_d[i * 128:(i + 1) * 128, :], zt[:], reads=[R_z], writes=[R_xg], add=True)
                k.op("dve", lambda e: e.memset(hnT[:, :, 0:3], 0.0), writes=[R_hn["halo"]])
                norm_phase([(xp[i * 128:(i + 1) * 128, :], 3 + i * 128, 128, i) for i in range(NCH)], grow, R_c, hnT, R_hn)
            with contextlib.ExitStack() as es:
                G = gate_prep(es, hnT, R_hn, wif, R_wif, bgrow, R_c)
                CP = {}
                wk = k.sb(es, "p_wk", [128, KD, 256], BF16); wv = k.sb(es, "p_wv", [128, KD, 256], BF16)
                kT = k.sb(es, "p_kT", [128, 2, NT], BF16)
                vaug_all = k.sb(es, "p_vaug", [128, NCH, VW], BF16); wv_all = k.sb(es, "p_wvall", [128, NCH, VW], BF16)
                ktok_all = k.sb(es, "p_ktok", [128, NCH, 256], BF16)
                R_wk = Res(); R_wv = Res(); R_kT = Res()
                R_vac = [Res() for _ in range(NCH)]; R_wvc = [Res() for _ in range(NCH)]; R_ktc = [Res() for _ in range(4)]
                R_Ch = [Res() for _ in range(4)]
                k.op("dve", lambda e: e.memset(vaug_all[:, :, 256:257], 1.0), writes=R_vac)
                for h in range(4):
                    load_w_cast(wk, w_in[:, C_K + h * 256:C_K + (h + 1) * 256], KD, R_wk)
                    load_w_cast(wv, w_in[:, C_V + h * 256:C_V + (h + 1) * 256], KD, R_wv)
                    convert_some(6)
                    for _ in conv_proj(es, f"pk{h}", wk, R_wk, 8 + 2 * h, hnT, R_hn, kT, R_kT, cw, cb, R_cw, 1.0 / 16.0, halo=False, CP=CP):
                        pass
                    batch_V(hnT, R_hn, wv, R_wv, None, None, vaug_all, R_vac, None, None)
                    batch_K(h, G, kT, R_kT, ktok_all, R_ktc, vaug_all, R_vac, wv_all, R_wvc)
                    kv_mm(0, ktok_all, R_ktc, wv_all, R_wvc)
                    for c in range(NCH):
                        if c + 1 < NCH:
                            kv_mm(c + 1, ktok_all, R_ktc, wv_all, R_wvc)
                        c_update(h, c, G, Cst, R_Ch[h])
                k.op("dve", lambda e: e.tensor_scalar_mul(out=Cst[:].rearrange("p a b c -> p (a b c)"),
                                                          in0=Cst[:].rearrange("p a b c -> p (a b c)"), scalar1=flag[:, 0:1]),
                     reads=R_Ch + [R_c, R_C], writes=[R_C])
                k.barrier()
            if dbg:
                o = ddbg("Cpre", [128, 4 * 2 * 257])
                k.dma("sp", o, Cst[:].rearrange("p a b c -> p (a b c)"), reads=[R_C], writes=[Res()])

            with contextlib.ExitStack() as es:
                grow = row_bcast(es, "gmixrow2", gmix_d, D, R_c)
                tiles = [(xh[:, :], 0, 3, "halo")] + [(xm[i * 128:(i + 1) * 128, :], 3 + i * 128, 128, i) for i in range(NCH)]
                norm_phase(tiles, grow, R_c, hnT, R_hn)
            if dbg:
                o = ddbg("hnT", [128, KD * HT], BF16)
                k.dma("sp", o, hnT[:].rearrange("p a b -> p (a b)"), reads=list(R_hn.values()), writes=[Res()])

            R_hg = Res("hgT_d")
            with contextlib.ExitStack() as es:
                wu = k.sb(es, "g_wu", [128, KD, 1024], BF16); wvg = k.sb(es, "g_wv", [128, KD, 1024], BF16)
                R_wu = Res(); R_wvg = Res()
                load_w_cast(wu, w_in[:, C_U:C_U + 1024], KD, R_wu, step=2)
                load_w_cast(wvg, w_in[:, C_VG:C_VG + 1024], KD, R_wvg, step=2)
                convert_some(8)
                lngrow = row_bcast(es, "lngrow", lng_d, 1024, R_c)
                lnbrow = row_bcast(es, "lnbrow", lnb_d, 1024, R_c)
                bsrow = row_bcast(es, "bsrow", bs_d, 1024, R_c)
                guT = [k.sb(es, f"g_guT{i}", [128, 8, 512], BF16) for i in range(2)]; R_gu = [Res(), Res()]
                gv = [k.sb(es, f"g_gv{i}", [128, 1024], F32) for i in range(3)]; R_gv = [Res(), Res(), Res()]
                tmp = [k.sb(es, f"g_tmp{i}", [128, 1024], F32) for i in range(2)]; R_tmp = [Res(), Res()]
                vn = [k.sb(es, f"g_vn{i}", [128, 1024], BF16) for i in range(2)]; R_vn = [Res(), Res()]
                stt = k.sb(es, "g_st", [128, 2, 6], F32); mv = k.sb(es, "g_mv", [128, 4], F32); R_st = Res()
                hgs_ = k.sb(es, "g_hgs", [128, 8, 512], BF16); hgs = [hgs_, hgs_]; R_hgs_ = Res(); R_hgs = [R_hgs_, R_hgs_]

                wsn = tmp[0][:].rearrange("p (g t) -> p g t", t=128); wsT = k.sb(es, "g_wsT", [128, 8, 128], BF16)
                R_ws = Res()
                k.dma("sp", wsn, ws_d.rearrange("g t s -> t g s"), writes=[R_tmp[0]])
                for g in range(8):
                    k.op("pe", TR(PS[0][:, 0:128], wsn[:, g, :], identf), reads=[R_tmp[0], R_c], writes=[RP[0]])
                    k.op("dve", lambda e: e.tensor_tensor(out=wsT[:, g, :], in0=PS[0][:, 0:128], in1=U, op=ALU.mult),
                         reads=[RP[0], R_c], writes=[R_ws])
                def stageU(st):
                    for g in range(8):
                        pb = 6 + (g % 2)
                        k.group("pe", [MM(PS[pb][:], wu[:, kk, g * 128:(g + 1) * 128], hnT[:, kk, 3 + st * 512:3 + (st + 1) * 512],
                                          kk == 0, kk == KD - 1) for kk in range(KD)],
                                reads=[R_hn[st * 4 + j] for j in range(4)] + [R_wu], writes=[RP[pb]])
                        k.op("act", lambda e: e.activation(out=guT[st % 2][:, g, :], in_=PS[pb][:], func=AF.Gelu_apprx_tanh),
                             reads=[RP[pb]], writes=[R_gu[st % 2]])

                def stageV(c):
                    b = c % 3
                    for nb in range(2):
                        k.group("pe", [MM(PS[nb][:], hnT[:, kk, 3 + c * 128:3 + (c + 1) * 128], wvg[:, kk, nb * 512:(nb + 1) * 512],
                                          kk == 0, kk == KD - 1) for kk in range(KD)], reads=[R_hn[c], R_wvg], writes=[RP[nb]])
                        k.op("act", lambda e: e.activation(out=gv[b][:, nb * 512:(nb + 1) * 512], in_=PS[nb][:], func=AF.Gelu_apprx_tanh),
                             reads=[RP[nb]], writes=[R_gv[b]])

                def stageM(c):
                    b = c % 2
                    b3 = c % 3
                    st = c // 4; cc = c % 4
                    for nb in range(2):
                        k.op("dve", lambda e: e.bn_stats(out=stt[:, nb, :], in_=gv[b3][:, nb * 512:(nb + 1) * 512]), reads=[R_gv[b3]], writes=[R_st])
                    k.op("dve", lambda e: e.bn_aggr(out=mv[:, 0:2], in_=stt[:].rearrange("p a b -> p (a b)")), reads=[R_st], writes=[R_st])
                    k.op("dve", lambda e: e.tensor_scalar_add(out=mv[:, 2:3], in0=mv[:, 1:2], scalar1=EPS), reads=[R_st], writes=[R_st])
                    k.op("pool", lambda e: e.tensor_tensor(out=mv[:, 2:3], in0=mv[:, 2:3], in1=mhalf[:, 0:1], op=ALU.pow), reads=[R_st, R_c], writes=[R_st])
                    k.op("dve", lambda e: e.tensor_scalar(out=tmp[b][:], in0=gv[b3][:], scalar1=mv[:, 0:1], scalar2=mv[:, 2:3],
                                                          op0=ALU.subtract, op1=ALU.mult), reads=[R_gv[b3], R_st], writes=[R_tmp[b]])
                    k.op("pool", lambda e: e.tensor_tensor(out=tmp[b][:], in0=tmp[b][:], in1=lngrow[:], op=ALU.mult), reads=[R_tmp[b], R_c], writes=[R_tmp[b]])
                    k.op("pool", lambda e: e.tensor_tensor(out=vn[b][:], in0=tmp[b][:], in1=lnbrow[:], op=ALU.add), reads=[R_tmp[b], R_c], writes=[R_vn[b]])
                    for g in range(8):
                        pb = 2 + g // 4
                        k.op("pe", MM(PS[pb][:, (g % 4) * 128:(g % 4 + 1) * 128], vn[b][:, g * 128:(g + 1) * 128], wsT[:, g, :], True, True),
                             reads=[R_vn[b], R_ws], writes=[RP[pb]])
                    for hb in range(2):
                        k.op("dve", lambda e: e.tensor_tensor(out=tmp[b][:, hb * 512:(hb + 1) * 512], in0=PS[2 + hb][:],
                                                              in1=bsrow[:, hb * 512:(hb + 1) * 512], op=ALU.add),
                             reads=[RP[2 + hb], R_c], writes=[R_tmp[b]])
                    sb_ = st % 2
                    k.op("dve", lambda e: e.tensor_tensor(out=hgs[sb_][:, :, cc * 128:(cc + 1) * 128],
                                                          in0=tmp[b][:].rearrange("p (g t) -> p g t", t=128),
                                                          in1=guT[sb_][:, :, cc * 128:(cc + 1) * 128], op=ALU.mult),
                         reads=[R_tmp[b], R_gu[sb_]], writes=[R_hgs[sb_]])
                    if cc == 3:
                        k.dma("sp", hgT_d[:, :, st * 512:(st + 1) * 512].rearrange("g p t -> p g t"), hgs[sb_][:],
                              reads=[R_hgs[sb_]], writes=[R_hg], add=True)

                stageU(0)
                stageV(0)
                stageV(1)
                for c in range(NCH):
                    if c % 4 == 1 and c // 4 + 1 < 4:
                        stageU(c // 4 + 1)
                    if c + 2 < NCH:
                        stageV(c + 2)
                    stageM(c)
                k.barrier()

            if stop_after == "gmlp":
                k.barrier()
                return nc, dbg_d

            R_hmd = Res("hmT_d")
            with contextlib.ExitStack() as es:
                G = gate_prep(es, hnT, R_hn, wif, R_wif, bgrow, R_c)
                CP = {}
                gnrow = row_bcast(es, "gnrow", gnm_d, 1024, R_c)
                scrA = k.sb(es, "m_scrA", [128, 2 * KD * 256], BF16); R_scrA = Res()
                wq = k.sb(es, "m_wq", [128, KD, 256], BF16); wk = k.sb(es, "m_wk", [128, KD, 256], BF16); R_wq = Res(); R_wk = Res()
                ktok_all = scrA[:, 0:NCH * 256].rearrange("p (a b) -> p a b", b=256)
                EB = scrA[:, 4096:4096 + 2048]
                DT = scrA[:, 6144:6144 + 2048].rearrange("p (a b) -> p a b", b=128)
                wv = k.sb(es, "m_wv", [128, KD, 256], BF16); wo = k.sb(es, "m_wo", [128, KD, 256], BF16)
                qT = k.sb(es, "m_qT", [128, 2, NT], BF16); kT = k.sb(es, "m_kT", [128, 2, NT], BF16)
                vaug_all = k.sb(es, "m_vaug", [128, NCH, VW], BF16); wv_all = k.sb(es, "m_wvall", [128, NCH, VW], BF16)
                sig_all = k.sb(es, "m_sig", [128, NCH, 256], BF16)
                num_all = k.sb(es, "m_num", [128, NCH, 260], F32)
                Ulf_ = k.sb(es, "m_Ulf", [128, 4, 128], F32); Ulf = [Ulf_, Ulf_]
                argm_ = k.sb(es, "m_argm", [128, 4, 128], F32); argm = [argm_, argm_]
                Cb2 = k.sb(es, "m_Cb2", [128, 2, 2, VW], BF16)
                pmx = k.sb(es, "m_pm", [128, 4, NCH], F32); st_all = k.sb(es, "m_stall", [128, NCH, 6], F32); mv_all = k.sb(es, "m_mvall", [128, NCH, 2], F32)
                hmTh = k.sb(es, "m_hmTh", [128, 2, NT], BF16)
                R_wv = Res(); R_wo = Res(); R_qT = Res(); R_kT = Res()
                R_vac = [Res() for _ in range(NCH)]; R_wvc = [Res() for _ in range(NCH)]; R_sigc = [Res() for _ in range(NCH)]
                R_numc = [Res() for _ in range(NCH)]; R_stc = [Res() for _ in range(NCH)]
                R_Ulf_ = Res(); R_Ulf = [R_Ulf_, R_Ulf_]; R_argm_ = Res(); R_argm = [R_argm_, R_argm_]; R_Cb2 = [Res(), Res()]; R_pm = Res(); R_hmTh = Res(); R_Ch = Res()
                R_ktc = [R_scrA] * 4
                k.op("dve", lambda e: e.memset(vaug_all[:, :, 256:257], 1.0), writes=R_vac)
                def loads_qk(h):
                    load_w_cast(wq, w_in[:, C_Q + h * 256:C_Q + (h + 1) * 256], KD, R_wq)
                    load_w_cast(wk, w_in[:, C_K + h * 256:C_K + (h + 1) * 256], KD, R_wk)

                def loads_vo(h):
                    load_w_cast(wv, w_in[:, C_V + h * 256:C_V + (h + 1) * 256], KD, R_wv)
                    load_w_cast(wo, w_in[:, C_O + h * 256:C_O + (h + 1) * 256], KD, R_wo)
                    convert_some(8)

                def conv_qk(h):
                    yield from conv_proj(es, f"mq{h}", wq, R_wq, 2 * h, hnT, R_hn, qT, R_qT, cw, cb, R_cw, 1.0, halo=True, CP=CP)
                    yield from conv_proj(es, f"mk{h}", wk, R_wk, 8 + 2 * h, hnT, R_hn, kT, R_kT, cw, cb, R_cw, 1.0 / 16.0, halo=True, CP=CP)

                def mid(h):
                    batch_V(hnT, R_hn, wv, R_wv, wo, R_wo, vaug_all, R_vac, sig_all, R_sigc)
                    k.start_fill(R_scrA)
                    for c4 in range(4):
                        ub = c4 % 2
                        for j in range(4):
                            c = c4 * 4 + j
                            k.op("dve", lambda e: e.tensor_scalar_mul(out=Ulf[ub][:, j, :], in0=U, scalar1=G["lf"][:, c, h:h + 1]),
                                 reads=[R_c, G["R"]], writes=[R_Ulf[ub]], add=(j > 0))
                        k.op("pe", MM(PS[2 + ub][:], ones, Ulf[ub][:].rearrange("p a b -> p (a b)"), True, True), reads=[R_Ulf[ub], R_c], writes=[RP[2 + ub]])
                        for j in range(4):
                            c = c4 * 4 + j
                            k.op("dve", lambda e: e.scalar_tensor_tensor(out=argm[ub][:, j, :], in0=PS[2 + ub][:, j * 128:(j + 1) * 128],
                                                                         scalar=G["biasc"][:, c, h:h + 1], in1=negmT, op0=ALU.add, op1=ALU.add),
                                 reads=[RP[2 + ub], G["R"], R_c], writes=[R_argm[ub]], add=(j > 0))
                        k.op("act", lambda e: e.activation(out=DT[:, c4 * 4:(c4 + 1) * 4, :], in_=argm[ub][:], func=AF.Exp),
                             reads=[R_argm[ub]], writes=[R_scrA], add=True)
                        k.op("act", lambda e: e.activation(out=EB[:, c4 * 512:(c4 + 1) * 512], in_=PS[2 + ub][:], func=AF.Exp),
                             reads=[RP[2 + ub]], writes=[R_scrA], add=True)
                    for c4 in range(4):
                        bank = 4 + c4 % 2
                        for j in range(4):
                            cs = slice((c4 * 4 + j) * 128, (c4 * 4 + j + 1) * 128)
                            k.group("pe", [MM(PS[bank][:, j * 128:(j + 1) * 128], kT[:, blk, cs], qT[:, blk, cs], blk == 0, blk == 1) for blk in range(2)],
                                    reads=[R_kT, R_qT], writes=[RP[bank]], add=(j > 0))
                        dtv = DT[:, c4 * 4:(c4 + 1) * 4, :]
                        k.op("dve", lambda e: e.tensor_tensor(out=dtv, in0=PS[bank][:].rearrange("p (a b) -> p a b", b=128), in1=dtv, op=ALU.mult),
                             reads=[RP[bank], R_scrA], writes=[R_scrA], add=True)
                    for c4 in range(4):
                        bank = 6 + c4 % 2
                        k.group("pe", [TR(PSB[bank][:, (j * 2 + blk) * 128:(j * 2 + blk + 1) * 128], kT[:, blk, (c4 * 4 + j) * 128:(c4 * 4 + j + 1) * 128], identb[:])
                                       for j in range(4) for blk in range(2)], reads=[R_kT, R_c], writes=[RP[bank]])
                        dst = ktok_all[:, c4 * 4:(c4 + 1) * 4, :]
                        src = PSB[bank][:, 0:1024].rearrange("p (a b) -> p a b", b=256)
                        if c4 % 2 == 0:
                            k.op("act", lambda e: e.copy(out=dst, in_=src), reads=[RP[bank]], writes=[R_scrA], add=True)
                        else:
                            k.op("dve", lambda e: e.tensor_copy(out=dst, in_=src), reads=[RP[bank]], writes=[R_scrA], add=True)
                    for c in range(NCH - 1):
                        k.op("dve", lambda e: e.tensor_scalar_mul(out=wv_all[:, c, 0:257], in0=vaug_all[:, c, 0:257], scalar1=G["wcol"][:, c, h:h + 1]),
                             reads=[R_vac[c], G["R"]], writes=[R_wvc[c]])
                    for blk in range(2):
                        k.op("dve", lambda e: e.tensor_tensor(out=qT[:, blk, :], in0=qT[:, blk, :], in1=EB, op=ALU.mult),
                             reads=[R_qT, R_scrA], writes=[R_qT])
                    k.op("act", lambda e: e.copy(out=Cb2[:, 0, :, 0:257], in_=Cst[:, h, :, :]), reads=[R_C, R_Ch], writes=[R_Cb2[0]])
                    kv_mm(0, ktok_all, R_ktc, wv_all, R_wvc)
                    for c in range(NCH):
                        cs = slice(c * 128, (c + 1) * 128)
                        par = c % 2
                        k.group("pe", [MM(PS[4 + par][:, 0:257], DT[:, c, :], vaug_all[:, c, 0:257], True, False),
                                       MM(PS[4 + par][:, 0:257], qT[:, 0, cs], Cb2[:, par, 0, 0:257], False, False),
                                       MM(PS[4 + par][:, 0:257], qT[:, 1, cs], Cb2[:, par, 1, 0:257], False, True)],
                                reads=[R_scrA, R_vac[c], R_qT, R_Cb2[par]], writes=[RP[4 + par]])
                        k.op("act", lambda e: e.copy(out=num_all[:, c, 0:257], in_=PS[4 + par][:, 0:257]), reads=[RP[4 + par]], writes=[R_numc[c]])
                        if c + 1 < NCH - 1:
                            kv_mm(c + 1, ktok_all, R_ktc, wv_all, R_wvc)
                        if c < NCH - 1:
                            c_update(h, c, G, Cst, R_Ch)
                            k.op("act", lambda e: e.copy(out=Cb2[:, 1 - par, :, 0:257], in_=Cst[:, h, :, :]), reads=[R_Ch], writes=[R_Cb2[1 - par]])

                def post(h):
                    den = num_all[:, :, 256]
                    k.op("dve", lambda e: e.scalar_tensor_tensor(out=pmx[:, 0, :], in0=den, scalar=-1.0, in1=den, op0=ALU.mult, op1=ALU.max),
                         reads=R_numc, writes=[R_pm])
                    k.op("dve", lambda e: e.tensor_scalar_max(out=pmx[:, 0, :], in0=pmx[:, 0, :], scalar1=1.0), reads=[R_pm], writes=[R_pm])
                    k.op("dve", lambda e: e.reciprocal(out=pmx[:, 1, :], in_=pmx[:, 0, :]), reads=[R_pm], writes=[R_pm])
                    yield
                    for c in range(NCH):
                        hv = num_all[:, c, 0:256]
                        k.op("act", lambda e: e.activation(out=hv, in_=hv, func=AF.Copy, scale=pmx[:, 1, c:c + 1]), reads=[R_numc[c], R_pm], writes=[R_numc[c]])
                        k.op("dve", lambda e: e.bn_stats(out=st_all[:, c, :], in_=hv), reads=[R_numc[c]], writes=[R_stc[c]])
                        k.op("dve", lambda e: e.bn_aggr(out=mv_all[:, c, :], in_=st_all[:, c, :]), reads=[R_stc[c]], writes=[R_stc[c]])
                        yield
                    k.op("dve", lambda e: e.tensor_scalar_add(out=pmx[:, 2, :], in0=mv_all[:, :, 1], scalar1=EPS), reads=R_stc + [R_pm], writes=[R_pm])
                    k.op("pool", lambda e: e.tensor_tensor(out=pmx[:, 2, :], in0=pmx[:, 2, :], in1=mhalf[:, 0:NCH], op=ALU.pow), reads=[R_pm, R_c], writes=[R_pm])
                    yield
                    k.start_fill(R_hmTh)
                    for c in range(NCH):
                        hv = num_all[:, c, 0:256]
                        k.op("dve", lambda e: e.tensor_scalar(out=hv, in0=hv, scalar1=mv_all[:, c, 0:1], scalar2=pmx[:, 2, c:c + 1],
                                                              op0=ALU.subtract, op1=ALU.mult), reads=[R_numc[c], R_stc[c], R_pm], writes=[R_numc[c]])
                        k.op("pool", lambda e: e.tensor_tensor(out=hv, in0=hv, in1=gnrow[:, h * 256:(h + 1) * 256], op=ALU.mult),
                             reads=[R_numc[c], R_c], writes=[R_numc[c]])
                        k.op("dve", lambda e: e.tensor_tensor(out=sig_all[:, c, :], in0=hv, in1=sig_all[:, c, :], op=ALU.mult),
                             reads=[R_numc[c], R_sigc[c]], writes=[R_sigc[c]])
                        bank = 4 + c % 2
                        cs = slice(c * 128, (c + 1) * 128)
                        k.group("pe", [TR(PSB[bank][:, blk * 128:(blk + 1) * 128], sig_all[:, c, blk * 128:(blk + 1) * 128], identb[:]) for blk in range(2)],
                                reads=[R_sigc[c], R_c], writes=[RP[bank]])
                        if c % 2 == 0:
                            k.op("act", lambda e: e.copy(out=hmTh[:, 0:2, cs], in_=PSB[bank][:, 0:256].rearrange("p (a t) -> p a t", t=128)),
                                 reads=[RP[bank]], writes=[R_hmTh], add=True)
                        else:
                            k.op("dve", lambda e: e.tensor_copy(out=hmTh[:, 0:2, cs], in_=PSB[bank][:, 0:256].rearrange("p (a t) -> p a t", t=128)),
                                 reads=[RP[bank]], writes=[R_hmTh], add=True)
                        yield
                    for blk in range(2):
                        k.dma("sp", hmT_d[2 * h + blk, :, :], hmTh[:, blk, :], reads=[R_hmTh], writes=[R_hmd], add=True)

                def adv1(g):
                    try:
                        next(g)
                        return True
                    except StopIteration:
                        return False

                loads_qk(0)
                loads_vo(0)
                for _ in conv_qk(0):
                    pass
                for h in range(4):
                    if h + 1 < 4:
                        loads_qk(h + 1)
                    mid(h)
                    gp = post(h)
                    if h + 1 < 4:
                        loads_vo(h + 1)
                        for _ in conv_qk(h + 1):
                            adv1(gp)
                    while adv1(gp):
                        pass
                k.barrier()
            if stop_after == "mlstm":
                k.barrier()
                return nc, dbg_d

            R_mg = Res("mgT_d")
            with contextlib.ExitStack() as es:
                hgT = k.sb(es, "t_hgT", [128, 8, NT], BF16); R_hgl = Res()
                for g in range(8):
                    k.dma("sp", hgT[:, g, :], hgT_d[g, :, :], reads=[R_hg], writes=[R_hgl], add=True)
                hmT = k.sb(es, "t_hmT", [128, 8, NT], BF16); R_hm = Res()
                for g in range(8):
                    k.dma("sp", hmT[:, g, :], hmT_d[g, :, :], reads=[R_hmd], writes=[R_hm], add=True)
                GW = 256
                wbm = [k.sb(es, f"t_wbm{i}", [128, 8, GW], BF16) for i in range(2)]
                wbg = [k.sb(es, f"t_wbg{i}", [128, 8, GW], BF16) for i in range(2)]
                wgm = [k.sb(es, f"t_wgm{i}", [128, KD, GW], BF16) for i in range(2)]
                wgg = [k.sb(es, f"t_wgg{i}", [128, KD, GW], BF16) for i in range(2)]
                R_w = [Res(), Res()]
                sgA = k.sb(es, "t_sgA", [128, 512], F32); sgD = k.sb(es, "t_sgD", [128, 512], F32)
                m1 = k.sb(es, "t_m1", [128, 512], F32); m2 = k.sb(es, "t_m2", [128, 512], F32)
                mst = [k.sb(es, f"t_mst{i}", [128, 512], BF16) for i in range(2)]
                R_sA = Res(); R_sD = Res(); R_m1 = Res(); R_m2 = Res(); R_ms = [Res(), Res()]

                def load_group(gi):
                    b = gi % 2
                    c0 = gi * GW
                    load_w_cast(wbm[b], wbm_d[:, c0:c0 + GW], 8, R_w[b], step=8)
                    load_w_cast(wbg[b], wbg_d[:, c0:c0 + GW], 8, R_w[b], step=8, new_fill=False)
                    load_w_cast(wgm[b], w_in[:, C_GM + c0:C_GM + c0 + GW], KD, R_w[b], step=8, new_fill=False)
                    load_w_cast(wgg[b], w_in[:, C_GG + c0:C_GG + c0 + GW], KD, R_w[b], step=8, new_fill=False)
                    convert_some(4)
                NG = D // GW
                load_group(0)
                it = 0
                for gi in range(NG):
                    if gi + 1 < NG:
                        load_group(gi + 1)
                    b = gi % 2
                    for jj in range(GW // 128):
                        j = gi * (GW // 128) + jj
                        js = slice(jj * 128, (jj + 1) * 128)
                        for tt in range(4):
                            ts_ = slice(tt * 512, (tt + 1) * 512)
                            hs_ = slice(3 + tt * 512, 3 + (tt + 1) * 512)
                            pa = 4 * (it % 2)
                            hn_reads = [R_hn[tt * 4 + q] for q in range(4)]
                            k.group("pe", [MM(PS[pa][:], wbm[b][:, kk, js], hmT[:, kk, ts_], kk == 0, kk == 7) for kk in range(8)],
                                    reads=[R_w[b], R_hm], writes=[RP[pa]])
                            k.group("pe", [MM(PS[pa + 1][:], wgm[b][:, kk, js], hnT[:, kk, hs_], kk == 0, kk == KD - 1) for kk in range(KD)],
                                    reads=[R_w[b]] + hn_reads, writes=[RP[pa + 1]])
                            k.group("pe", [MM(PS[pa + 2][:], wbg[b][:, kk, js], hgT[:, kk, ts_], kk == 0, kk == 7) for kk in range(8)],
                                    reads=[R_w[b], R_hgl], writes=[RP[pa + 2]])
                            k.group("pe", [MM(PS[pa + 3][:], wgg[b][:, kk, js], hnT[:, kk, hs_], kk == 0, kk == KD - 1) for kk in range(KD)],
                                    reads=[R_w[b]] + hn_reads, writes=[RP[pa + 3]])
                            k.op("act", lambda e: e.activation(out=sgA[:], in_=PS[pa + 1][:], func=AF.Sigmoid), reads=[RP[pa + 1]], writes=[R_sA])
                            k.op("act", lambda e: e.activation(out=sgD[:], in_=PS[pa + 3][:], func=AF.Sigmoid), reads=[RP[pa + 3]], writes=[R_sD])
                            k.op("dve", lambda e: e.tensor_tensor(out=m1[:], in0=PS[pa][:], in1=sgA[:], op=ALU.mult), reads=[RP[pa], R_sA], writes=[R_m1])
                            k.op("dve", lambda e: e.tensor_tensor(out=m2[:], in0=PS[pa + 2][:], in1=sgD[:], op=ALU.mult), reads=[RP[pa + 2], R_sD], writes=[R_m2])
                            mb = it % 2
                            k.op("pool", lambda e: e.tensor_tensor(out=mst[mb][:], in0=m1[:], in1=m2[:], op=ALU.add), reads=[R_m1, R_m2], writes=[R_ms[mb]])
                            k.dma("sp", mgT_d[j, :, ts_], mst[mb][:], reads=[R_ms[mb]], writes=[R_mg], add=True)
                            it += 1
                k.barrier()
        if stop_after == "tail1":
            k.barrier()
            return nc, dbg_d

        R_x1 = Res("x1_d")
        with contextlib.ExitStack() as es:
            mgT = k.sb(es, "u_mgT", [128, KD, NT], BF16); R_mgl = Res()
            for j in range(KD):
                k.dma("sp", mgT[:, j, :], mgT_d[j, :, :], reads=[R_mg], writes=[R_mgl], add=True)
            wout = k.sb(es, "u_wout", [128, KD, D], BF16); R_wo2 = Res()
            load_w_cast(wout, wout_d[:, :], KD, R_wo2, step=1)
            gfrow = row_bcast(es, "gffnrow", gffn_d, D, R_c)
            wr = k.sb(es, "u_wr", [128, KD, 36], F32); brrow = row_bcast(es, "brrow", br_d, 36, R_c)
            k.dma("sp", wr[:], wr_d.rearrange("(k p) c -> p k c", p=128), writes=[R_c], add=True)
            x1 = [k.sb(es, f"u_x1{i}", [128, D], F32) for i in range(2)]; R_x1t = [Res(), Res()]
            hn2 = [k.sb(es, f"u_hn2{i}", [128, D], F32) for i in range(3)]; R_h2 = [Res(), Res(), Res()]
            hn2b = [k.sb(es, f"u_hn2b{i}", [128, D], BF16) for i in range(3)]; R_h2b = [Res(), Res(), Res()]
            h2T = k.sb(es, "u_h2T", [128, KD, 128], F32); R_h2T = Res()
            ss = k.sb(es, "u_ss", [128, 2], F32); R_s = Res(); R_r = Res()
            L = k.sb(es, "u_L", [128, 36], F32); R_L = Res()
            rt = k.sb(es, "u_rt", [128, 16], F32); R_rt = Res()
            oh = k.sb(es, "u_oh", [128, 4], F32)
            msk = k.sb(es, "u_msk", [128, 32], F32); sel = k.sb(es, "u_sel", [128, 32], F32)
            oh1 = k.sb(es, "u_oh1", [128, 32], F32); oh2 = k.sb(es, "u_oh2", [128, 32], F32)
            m2_ = k.sb(es, "u_m2", [128, 32], F32); t32 = k.sb(es, "u_t32", [128, 32], F32)
            base = k.sb(es, "u_base", [128, 32], F32); spos = k.sb(es, "u_spos", [128, 32], F32)
            slf = k.sb(es, "u_slf", [128, 2], F32)
            sli = [k.sb(es, f"u_sli{i}", [128, 2], I32) for i in range(3)]; R_sli = [Res(), Res(), Res()]
            R_rr = Res("route")
            k.op("dve", lambda e: e.memset(base[:], 0.0), writes=[R_rr])
            def stageA(i):
                b = i % 2
                b3 = i % 3
                rows = slice(i * 128, (i + 1) * 128)
                k.dma("sp", x1[b][:], xm[rows, :], writes=[R_x1t[b]])
                for nb in range(4):
                    k.group("pe", [MM(PS[nb][:], mgT[:, kk, rows], wout[:, kk, nb * 512:(nb + 1) * 512], kk == 0, kk == KD - 1) for kk in range(KD)],
                            reads=[R_mgl, R_wo2], writes=[RP[nb]])
                    k.op("dve", lambda e: e.tensor_tensor(out=x1[b][:, nb * 512:(nb + 1) * 512], in0=PS[nb][:], in1=x1[b][:, nb * 512:(nb + 1) * 512], op=ALU.add),
                         reads=[RP[nb], R_x1t[b]], writes=[R_x1t[b]], add=True)
                    yield
                k.dma("sp", x1_d[rows, :], x1[b][:], reads=[R_x1t[b]], writes=[R_x1], add=True)
                k.op("act", lambda e: e.activation(out=hn2b[b3][:], in_=x1[b][:], func=AF.Square, accum_out=ss[:, 0:1]), reads=[R_x1t[b]], writes=[R_h2b[b3], R_s])
                rstd_from_ss(ss[:, 0:1], ss[:, 1:2], R_s, R_r, D)
                k.op("dve", lambda e: e.scalar_tensor_tensor(out=hn2[b3][:], in0=x1[b][:], scalar=ss[:, 1:2], in1=gfrow[:], op0=ALU.mult, op1=ALU.mult),
                     reads=[R_x1t[b], R_r, R_c], writes=[R_h2[b3]])
                k.op("act", lambda e: e.copy(out=hn2b[b3][:], in_=hn2[b3][:]), reads=[R_h2[b3]], writes=[R_h2b[b3]])

            def stageB(i):
                b = i % 3
                for q4 in range(4):
                    pb = 4 + (q4 % 2)
                    k.group("pe", [TR(PS[pb][:, j * 128:(j + 1) * 128], hn2[b][:, (q4 * 4 + j) * 128:(q4 * 4 + j + 1) * 128], identf) for j in range(4)],
                            reads=[R_h2[b], R_c], writes=[RP[pb]])
                    k.op("act", lambda e: e.copy(out=h2T[:, q4 * 4:(q4 + 1) * 4, :], in_=PS[pb][:].rearrange("p (a t) -> p a t", t=128)),
                         reads=[RP[pb]], writes=[R_h2T])
                yield
                k.group("pe", [MM(PS[6][:, 0:36], h2T[:, kk, :], wr[:, kk, :], kk == 0, kk == KD - 1) for kk in range(KD)],
                        reads=[R_h2T, R_c], writes=[RP[6]])
                k.op("dve", lambda e: e.tensor_tensor(out=L[:], in0=PS[6][:, 0:36], in1=brrow[:], op=ALU.add), reads=[RP[6], R_c], writes=[R_rr])
                yield

                def dv(fn):
                    k.op("dve", fn, reads=[R_rr, R_c], writes=[R_rr])

                def ac(fn):
                    k.op("act", fn, reads=[R_rr, R_c], writes=[R_rr])
                dv(lambda e: e.reduce_max(out=rt[:, 0:1], in_=L[:, 0:4], axis=mybir.AxisListType.X))
                dv(lambda e: e.tensor_scalar(out=oh[:], in0=L[:, 0:4], scalar1=rt[:, 0:1], scalar2=None, op0=ALU.is_equal))
                dv(lambda e: e.tensor_scalar_mul(out=rt[:, 1:2], in0=rt[:, 0:1], scalar1=-1.0))
                ac(lambda e: e.activation(out=t32[:, 0:4], in_=L[:, 0:4], func=AF.Exp, bias=rt[:, 1:2], accum_out=rt[:, 2:3]))
                dv(lambda e: e.reciprocal(out=rt[:, 3:4], in_=rt[:, 2:3]))
                dv(lambda e: e.tensor_scalar(out=oh[:], in0=oh[:], scalar1=1e4, scalar2=-1e4, op0=ALU.mult, op1=ALU.add))
                for gq in range(4):
                    dv(lambda e, gq=gq: e.tensor_scalar(out=msk[:, gq * 8:(gq + 1) * 8], in0=L[:, 4 + gq * 8:4 + (gq + 1) * 8],
                                                        scalar1=oh[:, gq:gq + 1], scalar2=None, op0=ALU.add))
                yield
                dv(lambda e: e.reduce_max(out=rt[:, 4:5], in_=msk[:], axis=mybir.AxisListType.X))
                dv(lambda e: e.tensor_scalar(out=oh1[:], in0=msk[:], scalar1=rt[:, 4:5], scalar2=None, op0=ALU.is_equal))
                dv(lambda e: e.scalar_tensor_tensor(out=m2_[:], in0=oh1[:], scalar=-3e4, in1=msk[:], op0=ALU.mult, op1=ALU.add))
                dv(lambda e: e.reduce_max(out=rt[:, 5:6], in_=m2_[:], axis=mybir.AxisListType.X))
                dv(lambda e: e.tensor_scalar(out=oh2[:], in0=m2_[:], scalar1=rt[:, 5:6], scalar2=None, op0=ALU.is_equal))
                dv(lambda e: e.tensor_tensor(out=sel[:], in0=oh1[:], in1=oh2[:], op=ALU.add))
                dv(lambda e: e.tensor_tensor(out=rt[:, 6:7], in0=rt[:, 5:6], in1=rt[:, 4:5], op=ALU.subtract))
                ac(lambda e: e.activation(out=rt[:, 7:8], in_=rt[:, 6:7], func=AF.Exp))
                dv(lambda e: e.tensor_scalar_add(out=rt[:, 8:9], in0=rt[:, 7:8], scalar1=1.0))
                dv(lambda e: e.reciprocal(out=rt[:, 8:9], in_=rt[:, 8:9]))
                k.op("dve", lambda e: e.tensor_tensor(out=wts_all[:, i, 0:1], in0=rt[:, 8:9], in1=rt[:, 3:4], op=ALU.mult), reads=[R_rr], writes=[R_rr, R_sw])
                k.op("dve", lambda e: e.tensor_tensor(out=wts_all[:, i, 1:2], in0=wts_all[:, i, 0:1], in1=rt[:, 7:8], op=ALU.mult), reads=[R_rr], writes=[R_rr, R_sw])
                yield
                k.op("pe", MM(PS[7][:, 0:32], Ustr, sel[:], True, True), reads=[R_rr, R_c], writes=[RP[7]])
                k.op("pe", MM(PS[7][:, 32:64], ones, sel[:], True, True), reads=[R_rr, R_c], writes=[RP[7]])
                k.op("dve", lambda e: e.tensor_tensor(out=spos[:], in0=PS[7][:, 0:32], in1=base[:], op=ALU.add), reads=[RP[7], R_rr], writes=[R_rr])
                k.op("dve", lambda e: e.tensor_tensor(out=base[:], in0=PS[7][:, 32:64], in1=base[:], op=ALU.add), reads=[RP[7], R_rr], writes=[R_rr])
                dv(lambda e: e.tensor_scalar(out=t32[:], in0=spos[:], scalar1=float(CAP), scalar2=1e6, op0=ALU.is_ge, op1=ALU.mult))
                dv(lambda e: e.tensor_tensor(out=spos[:], in0=spos[:], in1=t32[:], op=ALU.add))
                dv(lambda e: e.scalar_tensor_tensor(out=spos[:], in0=iota32, scalar=float(CAP), in1=spos[:], op0=ALU.mult, op1=ALU.add))
                for kk2, ohk in enumerate((oh1, oh2)):
                    dv(lambda e, ohk=ohk: e.tensor_tensor(out=t32[:], in0=ohk[:], in1=spos[:], op=ALU.mult))
                    dv(lambda e, kk2=kk2: e.reduce_sum(out=slf[:, kk2:kk2 + 1], in_=t32[:], axis=mybir.AxisListType.X))
                k.op("dve", lambda e: e.tensor_copy(out=sli[b][:], in_=slf[:]), reads=[R_rr], writes=[R_sli[b], R_rr])
                k.op("dve", lambda e: e.tensor_copy(out=slots_all[:, 2 * i:2 * i + 2], in_=sli[b][:]), reads=[R_sli[b]], writes=[R_sw])
                yield
                for kk2 in range(2):
                    k.dma("pool", None, None, reads=[R_h2b[b], R_sli[b]], writes=[R_xg], add=not (i == 0 and kk2 == 0),
                          fn=lambda e, kk2=kk2: e.indirect_dma_start(out=xg_d[:, :], out_offset=bass.IndirectOffsetOnAxis(ap=sli[b][:, kk2:kk2 + 1], axis=0),
                                                                     in_=hn2b[b][:, :], in_offset=None, bounds_check=bc_reg, oob_is_err=False))

            def adv(g, n=1):
                for _ in range(n):
                    try:
                        next(g)
                    except StopIteration:
                        return

            adv(stageA(0), 99)
            adv(stageA(1), 99)
            for i in range(NCH):
                ga = stageA(i + 2) if i + 2 < NCH else iter(())
                gb = stageB(i)
                adv(ga)
                adv(gb)
                adv(ga)
                adv(gb)
                adv(gb)
                adv(ga)
                adv(gb)
                adv(ga)
                adv(gb)
                adv(ga, 99)
                adv(gb, 99)
            k.barrier()
        if dbg:
            o = ddbg("slots", [128, NCH * 2], I32)
            k.dma("sp", o, slots_all[:], reads=[R_sw], writes=[Res()])
            o = ddbg("wts", [128, NCH * 2], F32)
            k.dma("sp", o, wts_all[:].rearrange("p a b -> p (a b)"), reads=[R_sw], writes=[Res()])
        if stop_after == "tail2":
            k.barrier()
            return nc, dbg_d

        convert_some(999)
        R_y = Res("y_d")
        with contextlib.ExitStack() as es:
            w1 = [k.sb(es, f"e_w1{i}", [128, KD, 512], BF16) for i in range(2)]
            w3 = [k.sb(es, f"e_w3{i}", [128, KD, 512], BF16) for i in range(2)]
            w2 = [k.sb(es, f"e_w2{i}", [128, 4, D], BF16) for i in range(2)]
            R_w = [Res(), Res()]
            X = [[k.sb(es, f"e_X{i}{j}", [128, D], BF16) for j in range(2)] for i in range(2)]
            R_X = [[Res(), Res()], [Res(), Res()]]
            XT = k.sb(es, "e_XT", [128, KD, 256], BF16); R_XT = Res()
            s1 = [k.sb(es, f"e_s1{i}", [128, 512], F32) for i in range(2)]; R_s1 = [Res(), Res()]
            actm = [k.sb(es, f"e_actm{i}", [128, 512], BF16) for i in range(2)]; R_am = [Res(), Res()]
            actT = k.sb(es, "e_actT", [128, 4, 256], BF16); R_at = [Res(), Res()]
            ys = [k.sb(es, f"e_ys{i}", [128, D], F32) for i in range(2)]; R_ys = [Res(), Res()]

            def load_e(e_):
                b = e_ % 2
                k.start_fill(R_w[b])
                for (dst_, src_, kc) in ((w1[b], w1b_d[e_], KD), (w3[b], w3b_d[e_], KD), (w2[b], w2b_d[e_], 4)):
                    v_ = src_.rearrange("(k p) c -> p k c", p=128)
                    hk = kc // 2
                    for k0 in (0, hk):
                        k.dma("sp", dst_[:, k0:k0 + hk, :], v_[:, k0:k0 + hk, :], reads=[R_wb], writes=[R_w[b]], add=True)
                for blk in range(2):
                    r0 = e_ * CAP + blk * 128
                    k.dma("sp", X[b][blk][:], xg_d[r0:r0 + 128, :], reads=[R_xg], writes=[R_X[b][blk]])
            load_e(0)
            yi = 0
            for e_ in range(32):
                if e_ + 1 < 32:
                    load_e(e_ + 1)
                b = e_ % 2
                for blk in range(2):
                    for hb in range(2):
                        pb = PSB[4 + hb]
                        k.group("pe", [TR(pb[:, j * 128:(j + 1) * 128], X[b][blk][:, (hb * 8 + j) * 128:(hb * 8 + j + 1) * 128], identb[:]) for j in range(8)],
                                reads=[R_X[b][blk], R_c], writes=[RP[4 + hb]])
                        src3 = pb.rearrange("p (a t) -> p a t", t=128)
                        dst3 = XT[:, hb * 8:(hb + 1) * 8, blk * 128:(blk + 1) * 128]
                        if hb == 0:
                            k.op("act", lambda e: e.copy(out=dst3, in_=src3), reads=[RP[4 + hb]], writes=[R_XT])
                        else:
                            k.op("dve", lambda e: e.tensor_copy(out=dst3, in_=src3), reads=[RP[4 + hb]], writes=[R_XT])
                for blk in range(2):
                    pa, pc_ = (2, 3) if blk == 0 else (6, 7)
                    bs = slice(blk * 128, (blk + 1) * 128)
                    k.group("pe", [MM(PS[pa][:], XT[:, kk, bs], w1[b][:, kk, :], kk == 0, kk == KD - 1) for kk in range(KD)],
                            reads=[R_w[b], R_XT], writes=[RP[pa]])
                    k.group("pe", [MM(PS[pc_][:], XT[:, kk, bs], w3[b][:, kk, :], kk == 0, kk == KD - 1) for kk in range(KD)],
                            reads=[R_w[b], R_XT], writes=[RP[pc_]])
                    k.op("act", lambda e: e.activation(out=s1[blk][:], in_=PS[pa][:], func=AF.Silu), reads=[RP[pa]], writes=[R_s1[blk]])
                    k.op("dve", lambda e: e.tensor_tensor(out=actm[blk][:], in0=PS[pc_][:], in1=s1[blk][:], op=ALU.mult),
                         reads=[RP[pc_], R_s1[blk]], writes=[R_am[blk]])
                    k.group("pe", [TR(PSB[4 + blk][:, fb * 128:(fb + 1) * 128], actm[blk][:, fb * 128:(fb + 1) * 128], identb[:]) for fb in range(4)],
                            reads=[R_am[blk], R_c], writes=[RP[4 + blk]])
                    src3 = PSB[4 + blk][:, 0:512].rearrange("p (a t) -> p a t", t=128)
                    if blk == 0:
                        k.op("act", lambda e: e.copy(out=actT[:, :, bs], in_=src3), reads=[RP[4 + blk]], writes=[R_at[blk]])
                    else:
                        k.op("dve", lambda e: e.tensor_copy(out=actT[:, :, bs], in_=src3), reads=[RP[4 + blk]], writes=[R_at[blk]])
                for blk in range(2):
                    yb = yi % 2
                    for nb in range(4):
                        pn = nb % 2
                        k.group("pe", [MM(PS[pn][:], actT[:, fb, blk * 128:(blk + 1) * 128], w2[b][:, fb, nb * 512:(nb + 1) * 512], fb == 0, fb == 3) for fb in range(4)],
                                reads=[R_at[blk], R_w[b]], writes=[RP[pn]])
                        if pn == 0:
                            k.op("act", lambda e: e.copy(out=ys[yb][:, nb * 512:(nb + 1) * 512], in_=PS[pn][:]), reads=[RP[pn]], writes=[R_ys[yb]], add=(nb > 0))
                        else:
                            k.op("dve", lambda e: e.tensor_copy(out=ys[yb][:, nb * 512:(nb + 1) * 512], in_=PS[pn][:]), reads=[RP[pn]], writes=[R_ys[yb]], add=True)
                    r0 = e_ * CAP + blk * 128
                    k.dma("sp", y_d[r0:r0 + 128, :], ys[yb][:], reads=[R_ys[yb]], writes=[R_y], add=True)
                    yi += 1
            k.barrier()
        if stop_after == "experts":
            k.barrier()
            return nc, dbg_d

        R_out = Res("out")
        with contextlib.ExitStack() as es:
            wpg = k.sb(es, "f_wpg", [128, KD, D], BF16); wpu = k.sb(es, "f_wpu", [128, 2, D], BF16); R_wp = Res()
            load_w_cast(wpg, wpg_d[:, :], KD, R_wp, step=1)
            load_w_cast(wpu, wpu_d[:, :], 2, R_wp, step=1, new_fill=False)
            gprow = row_bcast(es, "gplerow", gple_d, D, R_c)
            gfrow = row_bcast(es, "gfinrow", gfin_d, D, R_c)
            x1t = [k.sb(es, f"f_x1{i}", [128, D], F32) for i in range(2)]; R_x1t = [Res(), Res()]
            x2t = [k.sb(es, f"f_x2{i}", [128, D], F32) for i in range(3)]; R_x2t = [Res(), Res(), Res()]
            ya = [k.sb(es, f"f_ya{i}", [128, D], F32) for i in range(2)]; R_ya = [Res(), Res()]
            yb_ = [k.sb(es, f"f_yb{i}", [128, D], F32) for i in range(2)]; R_yb = [Res(), Res()]
            pt = [k.sb(es, f"f_pt{i}", [128, 256], F32) for i in range(2)]; R_pt = [Res(), Res()]
            ptb = [k.sb(es, f"f_ptb{i}", [128, 256], BF16) for i in range(2)]; R_ptb = [Res(), Res()]
            pT = [k.sb(es, f"f_pT{i}", [128, 2, 128], BF16) for i in range(2)]; R_pT = [Res(), Res()]
            hb3 = [k.sb(es, f"f_hb3{i}", [128, D], BF16) for i in range(2)]; R_hb3 = [Res(), Res()]
            h3T = [k.sb(es, f"f_h3T{i}", [128, KD, 128], BF16) for i in range(2)]; R_h3T = [Res(), Res()]
            sg = [k.sb(es, f"f_sg{i}", [128, 512], F32) for i in range(2)]; R_sg = [Res(), Res()]
            tq = [k.sb(es, f"f_tq{i}", [128, 512], F32) for i in range(2)]; R_tq = [Res(), Res()]
            x3 = k.sb(es, "f_x3", [128, D], F32); R_x3 = Res()
            ot_ = k.sb(es, "f_ot", [128, D], F32); ot = [ot_, ot_]; R_ot_ = Res(); R_ot = [R_ot_, R_ot_]
            ssA = k.sb(es, "f_ssA", [128, 2], F32); R_sA = Res(); R_rA = Res()
            ssB = k.sb(es, "f_ssB", [128, 2], F32); R_sB = Res(); R_rB = Res()

            def stageG(i):
                b = i % 2
                rows = slice(i * 128, (i + 1) * 128)
                k.dma("sp", x1t[b][:], x1_d[rows, :], reads=[R_x1], writes=[R_x1t[b]])
                k.dma("sp", pt[b][:], pm[rows, :], writes=[R_pt[b]])
                k.op("pool", lambda e: e.memset(ya[b][:], 0.0), writes=[R_ya[b]])
                k.op("pool", lambda e: e.memset(yb_[b][:], 0.0), writes=[R_yb[b]])
                for kk2, (yt, Ry) in enumerate(((ya[b], R_ya[b]), (yb_[b], R_yb[b]))):
                    k.dma("pool", None, None, reads=[R_y, R_sw, Ry], writes=[Ry], add=True,
                          fn=lambda e, kk2=kk2, yt=yt: e.indirect_dma_start(out=yt[:, :], out_offset=None, in_=y_d[:, :],
                                                                           in_offset=bass.IndirectOffsetOnAxis(ap=slots_all[:, 2 * i + kk2:2 * i + kk2 + 1], axis=0),
                                                                           bounds_check=bc_reg, oob_is_err=False))

            def A1(i):
                b = i % 2
                t3 = i % 3
                k.op("dve", lambda e: e.scalar_tensor_tensor(out=x2t[t3][:], in0=ya[b][:], scalar=wts_all[:, i, 0:1], in1=x1t[b][:], op0=ALU.mult, op1=ALU.add),
                     reads=[R_ya[b], R_sw, R_x1t[b]], writes=[R_x2t[t3]])
                k.op("dve", lambda e: e.scalar_tensor_tensor(out=x2t[t3][:], in0=yb_[b][:], scalar=wts_all[:, i, 1:2], in1=x2t[t3][:], op0=ALU.mult, op1=ALU.add),
                     reads=[R_yb[b], R_sw, R_x2t[t3]], writes=[R_x2t[t3]])
                if dbg and i == 0:
                    o = ddbg("x2t0", [128, D])
                    k.dma("sp", o, x2t[t3][:], reads=[R_x2t[t3]], writes=[Res()])

            def A2(i):
                b = i % 2
                t3 = i % 3
                k.op("act", lambda e: e.activation(out=hb3[b][:], in_=x2t[t3][:], func=AF.Square, accum_out=ssA[:, 0:1]), reads=[R_x2t[t3]], writes=[R_hb3[b], R_sA])
                rstd_from_ss(ssA[:, 0:1], ssA[:, 1:2], R_sA, R_rA, D)
                k.op("dve", lambda e: e.scalar_tensor_tensor(out=hb3[b][:], in0=x2t[t3][:], scalar=ssA[:, 1:2], in1=gprow[:], op0=ALU.mult, op1=ALU.mult),
                     reads=[R_x2t[t3], R_rA, R_c], writes=[R_hb3[b]])
                k.op("dve", lambda e: e.tensor_copy(out=ptb[b][:], in_=pt[b][:]), reads=[R_pt[b]], writes=[R_ptb[b]])

            def A3(i):
                b = i % 2
                t3 = i % 3
                for hb in range(2):
                    pb = PSB[4 + hb]
                    k.group("pe", [TR(pb[:, j * 128:(j + 1) * 128], hb3[b][:, (hb * 8 + j) * 128:(hb * 8 + j + 1) * 128], identb[:]) for j in range(8)],
                            reads=[R_hb3[b], R_c], writes=[RP[4 + hb]])
                    k.op("act", lambda e: e.copy(out=h3T[b][:, hb * 8:(hb + 1) * 8, :], in_=pb.rearrange("p (a t) -> p a t", t=128)),
                         reads=[RP[4 + hb]], writes=[R_h3T[b]])
                k.group("pe", [TR(PSB[6][:, j * 128:(j + 1) * 128], ptb[b][:, j * 128:(j + 1) * 128], identb[:]) for j in range(2)],
                        reads=[R_ptb[b], R_c], writes=[RP[6]])
                k.op("dve", lambda e: e.tensor_copy(out=pT[b][:], in_=PSB[6][:, 0:256].rearrange("p (a t) -> p a t", t=128)), reads=[RP[6]], writes=[R_pT[b]])

            def Bnb(i, nb):
                b = i % 2
                t3 = i % 3
                ns = slice(nb * 512, (nb + 1) * 512)
                pg = nb % 2
                gb_ = (0, 1, 7)[(i * 4 + nb) % 3]
                k.group("pe", [MM(PS[gb_][:], h3T[b][:, kk, :], wpg[:, kk, ns], kk == 0, kk == KD - 1) for kk in range(KD)],
                        reads=[R_h3T[b], R_wp], writes=[RP[gb_]])
                k.group("pe", [MM(PS[2 + pg][:], pT[b][:, kk, :], wpu[:, kk, ns], kk == 0, kk == 1) for kk in range(2)],
                        reads=[R_pT[b], R_wp], writes=[RP[2 + pg]])
                k.op("act", lambda e: e.activation(out=sg[pg][:], in_=PS[gb_][:], func=AF.Sigmoid), reads=[RP[gb_]], writes=[R_sg[pg]])
                k.op("dve", lambda e: e.tensor_tensor(out=tq[pg][:], in0=PS[2 + pg][:], in1=sg[pg][:], op=ALU.mult), reads=[RP[2 + pg], R_sg[pg]], writes=[R_tq[pg]])
                k.op("pool", lambda e: e.tensor_tensor(out=x3[:, ns], in0=tq[pg][:], in1=x2t[t3][:, ns], op=ALU.add), reads=[R_tq[pg], R_x2t[t3]], writes=[R_x3])

            def Bfin(i):
                b = i % 2
                rows = slice(i * 128, (i + 1) * 128)
                k.op("act", lambda e: e.activation(out=ot[b][:], in_=x3[:], func=AF.Square, accum_out=ssB[:, 0:1]), reads=[R_x3], writes=[R_ot[b], R_sB])
                rstd_from_ss(ssB[:, 0:1], ssB[:, 1:2], R_sB, R_rB, D)
                k.op("dve", lambda e: e.scalar_tensor_tensor(out=ot[b][:], in0=x3[:], scalar=ssB[:, 1:2], in1=gfrow[:], op0=ALU.mult, op1=ALU.mult),
                     reads=[R_x3, R_rB, R_c], writes=[R_ot[b]])
                k.dma("sp", out_d[rows, :], ot[b][:], reads=[R_ot[b]], writes=[R_out], add=True)

            stageG(0); stageG(1)
            A1(0); A2(0); A3(0)
            stageG(2)
            A1(1); A2(1)
            for i in range(NCH):
                Bnb(i, 0)
                if i + 2 < NCH:
                    A1(i + 2)
                Bnb(i, 1)
                if i + 2 < NCH:
                    A2(i + 2)
                Bnb(i, 2)
                if i + 3 < NCH:
                    stageG(i + 3)
                if i + 1 < NCH:
                    A3(i + 1)
                Bnb(i, 3)
                Bfin(i)
            k.barrier()
    return nc, dbg_d


def host_consts():
    c = np.zeros((128, 6, 128), np.float32)
    s = np.arange(128)[:, None]; t = np.arange(128)[None, :]
    c[:, 0, :] = np.eye(128)
    c[:, 1, :] = (s <= t)
    c[:, 2, :] = 1.0
    c[:, 3, :] = np.where(s <= t, 0.0, -30000.0)
    c[:, 4, :] = (s < t)
    c[:, 5, :] = np.arange(128)[None, :]
    return c


def make_in_maps(inp, cores=range(8)):
    x = np.asarray(inp["x"], np.float32); p = np.asarray(inp["p"], np.float32)[0]
    g = lambda n: np.ascontiguousarray(np.asarray(inp[n], np.float32)[0])
    conv_w = g("conv_w"); conv_b = g("conv_b")
    shared = {
        "w_in": g("w_in"),
        "convw": np.ascontiguousarray(conv_w.reshape(4, 16, 128).transpose(2, 1, 0)),
        "convb": np.ascontiguousarray(conv_b.reshape(16, 128).T),
        "b_gate": g("b_gate"), "gn_m": g("gn_m"), "ln_g": g("ln_g"), "ln_b": g("ln_b"),
        "w_s": g("w_s"), "b_s": np.ascontiguousarray(g("b_s").reshape(1024)),
        "w_bm": g("w_bm"), "w_bg": g("w_bg"), "w_out": g("w_out"),
        "g_mix": g("g_mix"), "g_ffn": g("g_ffn"), "g_ple": g("g_ple"),
        "g_final": np.ascontiguousarray(np.asarray(inp["g_final"], np.float32)),
        "w_r": np.ascontiguousarray(np.concatenate([g("w_rg"), g("w_re")], axis=1)),
        "b_r": np.ascontiguousarray(np.concatenate([g("b_rg"), g("b_re")], axis=0)),
        "w1": g("w1"), "w3": g("w3"), "w2": g("w2"),
        "w_ple_up": g("w_ple_up"), "w_ple_gate": g("w_ple_gate"),
        "consts": host_consts(),
    }
    maps = []
    for c in cores:
        b, half = c // 2, c % 2
        m = dict(shared)
        m["xm"] = np.ascontiguousarray(x[b, half * NT:(half + 1) * NT])
        m["xp"] = np.ascontiguousarray(x[b, 0:NT])
        m["xh"] = np.ascontiguousarray(x[b, NT - 128:NT]) if half == 1 else np.zeros((128, D), np.float32)
        m["flag"] = np.full((128, 1), float(half), np.float32)
        m["pm"] = np.ascontiguousarray(p[b, half * NT:(half + 1) * NT])
        maps.append(m)
    return maps


def kernel(**inputs):
    nc, _ = build()
    maps = make_in_maps(inputs)
    res = run_bass_kernel_spmd(nc, maps, core_ids=list(range(8)))
    out = np.zeros((4, 4096, D), np.float32)
    for c in range(8):
        b, half = c // 2, c % 2
        out[b, half * NT:(half + 1) * NT] = np.asarray(res.results[c]["out"], np.float32)
    return out
```
